# Optimizing a Trainium2 kernel written in Bass

```python
import math
import jax
import jax.numpy as jnp
from jax import lax
import numpy as np

D_MODEL = 2048
BATCH = 8
SEQ = 2048
DEPTH = 4

GRID_W = 64
CTX_LEN = 256
N_EVEN = (DEPTH + 1) // 2
N_ODD = DEPTH // 2

Q_BLOCK = 128
ROPE_THETA = 10000.0
NORM_EPS = 1e-6
NEG_INF = -1e30

A_HEADS = 8
A_KV_HEADS = 2
A_HEAD_DIM = 128
A_Q_W = A_HEADS * A_HEAD_DIM
A_KV_W = A_KV_HEADS * A_HEAD_DIM
S5_W = 1024
S5_GROUP = 16
S5_GROUPS = S5_W // S5_GROUP
S5_STATE = 64
C_HEADS = 16
C_KV_HEADS = 2
C_HEAD_DIM = 64
C_Q_W = C_HEADS * C_HEAD_DIM
C_KV_W = C_KV_HEADS * C_HEAD_DIM
C_WINDOW = 128
M_INNER = 1024
M_HEAD_DIM = 64
M_HEADS = M_INNER // M_HEAD_DIM
M_GROUPS = 2
M_STATE = 128
M_CONV = 3
M_CHUNK = 128
M_XBC = M_INNER + 2 * M_GROUPS * M_STATE

EVEN_SPLITS = (A_Q_W, A_KV_W, A_KV_W, S5_W)
ODD_SPLITS = (C_Q_W, C_KV_W, C_KV_W, M_INNER, M_XBC, 2 * M_HEADS)
EVEN_IN = sum(EVEN_SPLITS)
ODD_IN = sum(ODD_SPLITS)
EVEN_MIX = A_Q_W + S5_W
ODD_MIX = C_Q_W + M_INNER

N_EXPERTS = 16
EXPERT_FF = 1024
CAPACITY_FACTOR = 2

kernel_name = "hybrid_diffusion_trunk"


def rms_norm(x, g):
    xf = x.astype(jnp.float32)
    y = xf * lax.rsqrt(jnp.mean(xf * xf, axis=-1, keepdims=True) + NORM_EPS)
    return (y * g.astype(jnp.float32)).astype(x.dtype)


def split_cols(p, widths):
    return jnp.split(p, np.cumsum(widths)[:-1].tolist(), axis=-1)


def axial_rope_tables(n_tokens, head_dim):
    rows = n_tokens // GRID_W
    n_freq = head_dim // 4
    inv = ROPE_THETA ** (-jnp.arange(n_freq, dtype=jnp.float32) / n_freq)
    row = jnp.repeat(jnp.arange(rows, dtype=jnp.float32), GRID_W)
    col = jnp.tile(jnp.arange(GRID_W, dtype=jnp.float32), rows)
    ang = jnp.concatenate([row[:, None] * inv, col[:, None] * inv], axis=-1)
    return jnp.cos(ang), jnp.sin(ang)


def apply_rope(x, cos, sin):
    half = x.shape[-1] // 2
    x1, x2 = x[..., :half], x[..., half:]
    cos = cos[None, :, None, :].astype(x.dtype)
    sin = sin[None, :, None, :].astype(x.dtype)
    return jnp.concatenate([x1 * cos - x2 * sin, x1 * sin + x2 * cos], axis=-1)


def gqa_logits(q, k):
    scale = q.shape[-1] ** -0.5
    return jnp.einsum('bqgrd,bkgd->bgrqk', q, k, preferred_element_type=jnp.float32) * scale


def gqa_values(p, v):
    return jnp.einsum('bgrqk,bkgd->bqgrd', p.astype(v.dtype), v)


def mixer_a(q_c, k_c, v_c, q_x, k_x, v_x, q_norm_g, k_norm_g, with_ctx_out):
    bsz, n_lat = q_x.shape[:2]
    n_ctx = q_c.shape[1]
    rep = A_HEADS // A_KV_HEADS

    def heads(q, k, v):
        n = q.shape[1]
        q = rms_norm(q.reshape(bsz, n, A_HEADS, A_HEAD_DIM), q_norm_g)
        k = rms_norm(k.reshape(bsz, n, A_KV_HEADS, A_HEAD_DIM), k_norm_g)
        return q, k, v.reshape(bsz, n, A_KV_HEADS, A_HEAD_DIM)

    qc, kc, vc = heads(q_c, k_c, v_c)
    qx, kx, vx = heads(q_x, k_x, v_x)
    cos, sin = axial_rope_tables(n_lat, A_HEAD_DIM)
    qx = apply_rope(qx, cos, sin)
    kx = apply_rope(kx, cos, sin)
    k_all = jnp.concatenate([kc, kx], axis=1)
    v_all = jnp.concatenate([vc, vx], axis=1)
    nb = n_lat // Q_BLOCK
    qb = qx.reshape(bsz, nb, Q_BLOCK, A_KV_HEADS, rep, A_HEAD_DIM).swapaxes(0, 1)

    def block(q):
        return gqa_values(jax.nn.softmax(gqa_logits(q, k_all), axis=-1), v_all)

    out_x = lax.map(block, qb).swapaxes(0, 1).reshape(bsz, n_lat, A_Q_W)
    out_c = None
    if with_ctx_out:
        qcg = qc.reshape(bsz, n_ctx, A_KV_HEADS, rep, A_HEAD_DIM)
        out_c = gqa_values(jax.nn.softmax(gqa_logits(qcg, kc), axis=-1), vc).reshape(bsz, n_ctx, A_Q_W)
    return out_c, out_x


def _linear_combine(e1, e2):
    a1, b1 = e1
    a2, b2 = e2
    return a1 * a2, a2 * b1 + b2


def s5_discretise(a_re, a_im, log_dt, b_re, b_im):
    lam = lax.complex(a_re.astype(jnp.float32), a_im.astype(jnp.float32))
    dt = jnp.exp(log_dt.astype(jnp.float32))[:, None]
    lam_bar = jnp.exp(lam * dt)
    b = lax.complex(b_re.astype(jnp.float32), b_im.astype(jnp.float32))
    b_bar = ((lam_bar - 1.0) / lam)[..., None] * b
    return lam_bar, b_bar


def s5_scan(u, lam_bar, b_bar, h0, reverse):
    bu = jnp.einsum('gph,blgh->blgp', b_bar, u.astype(jnp.complex64))
    if h0 is not None:
        edge = -1 if reverse else 0
        bu = bu.at[:, edge].add(lam_bar * h0)
    a = jnp.broadcast_to(lam_bar, bu.shape)
    _, h = lax.associative_scan(_linear_combine, (a, bu), reverse=reverse, axis=1)
    return h


def mixer_b(u_c, u_x, a_re, a_im, log_dt, b_re, b_im, c_re, c_im, d_skip, glu_w, glu_b, with_ctx_out):
    bsz, n_lat = u_x.shape[:2]
    n_ctx = u_c.shape[1]
    uc = u_c.astype(jnp.float32).reshape(bsz, n_ctx, S5_GROUPS, S5_GROUP)
    ux = u_x.astype(jnp.float32).reshape(bsz, n_lat, S5_GROUPS, S5_GROUP)
    ys_x, ys_c = [], []
    for direction in range(2):
        reverse = direction == 1
        lam_bar, b_bar = s5_discretise(a_re[direction], a_im[direction], log_dt[direction],
                                       b_re[direction], b_im[direction])
        cm = lax.complex(c_re[direction].astype(jnp.float32), c_im[direction].astype(jnp.float32))
        h_c = s5_scan(uc, lam_bar, b_bar, None, reverse)
        h_end = h_c[:, 0] if reverse else h_c[:, -1]
        h_x = s5_scan(ux, lam_bar, b_bar, h_end, reverse)
        ys_x.append(jnp.einsum('ghp,blgp->blgh', cm, h_x).real)
        if with_ctx_out:
            ys_c.append(jnp.einsum('ghp,blgp->blgh', cm, h_c).real)

    def readout(y, u_in):
        y = y.reshape(u_in.shape).astype(u_in.dtype) + d_skip * u_in
        g = jax.nn.gelu(y)
        return g * jax.nn.sigmoid(g @ glu_w + glu_b)

    out_c = readout(ys_c[0] + ys_c[1], u_c) if with_ctx_out else None
    return out_c, readout(ys_x[0] + ys_x[1], u_x)


def mixer_c(q_c, k_c, v_c, q_x, k_x, v_x, sink, with_ctx_out):
    bsz, n_lat = q_x.shape[:2]
    n_ctx = q_c.shape[1]
    rep = C_HEADS // C_KV_HEADS
    kc = k_c.reshape(bsz, n_ctx, C_KV_HEADS, C_HEAD_DIM)
    vc = v_c.reshape(bsz, n_ctx, C_KV_HEADS, C_HEAD_DIM)
    cos, sin = axial_rope_tables(n_lat, C_HEAD_DIM)
    qx = apply_rope(q_x.reshape(bsz, n_lat, C_HEADS, C_HEAD_DIM), cos, sin)
    qx = qx.reshape(bsz, n_lat, C_KV_HEADS, rep, C_HEAD_DIM)
    kx = apply_rope(k_x.reshape(bsz, n_lat, C_KV_HEADS, C_HEAD_DIM), cos, sin)
    vx = v_x.reshape(bsz, n_lat, C_KV_HEADS, C_HEAD_DIM)
    sink_g = sink.astype(jnp.float32).reshape(C_KV_HEADS, rep)[None, :, :, None, None]

    def sink_softmax(s):
        sk = jnp.broadcast_to(sink_g, s.shape[:-1] + (1,))
        return jax.nn.softmax(jnp.concatenate([s, sk], axis=-1), axis=-1)[..., :-1]

    span = Q_BLOCK + 2 * C_WINDOW
    pad = ((0, 0), (C_WINDOW, C_WINDOW), (0, 0), (0, 0))
    kp = jnp.pad(kx, pad)
    vp = jnp.pad(vx, pad)
    nb = n_lat // Q_BLOCK
    qb = qx.reshape(bsz, nb, Q_BLOCK, C_KV_HEADS, rep, C_HEAD_DIM).swapaxes(0, 1)
    starts = jnp.arange(nb, dtype=jnp.int32) * Q_BLOCK
    q_off = jnp.arange(Q_BLOCK, dtype=jnp.int32)
    k_off = jnp.arange(span, dtype=jnp.int32) - C_WINDOW

    def block(args):
        start, q = args
        kw = lax.dynamic_slice_in_dim(kp, start, span, axis=1)
        vw = lax.dynamic_slice_in_dim(vp, start, span, axis=1)
        kpos = start + k_off
        valid = ((jnp.abs(k_off[None, :] - q_off[:, None]) <= C_WINDOW)
                 & (kpos >= 0)[None, :] & (kpos < n_lat)[None, :])
        s_w = jnp.where(valid, gqa_logits(q, kw), NEG_INF)
        p = sink_softmax(jnp.concatenate([gqa_logits(q, kc), s_w], axis=-1))
        return gqa_values(p[..., :n_ctx], vc) + gqa_values(p[..., n_ctx:], vw)

    out_x = lax.map(block, (starts, qb)).swapaxes(0, 1).reshape(bsz, n_lat, C_Q_W)
    out_c = None
    if with_ctx_out:
        qc = q_c.reshape(bsz, n_ctx, C_KV_HEADS, rep, C_HEAD_DIM)
        out_c = gqa_values(sink_softmax(gqa_logits(qc, kc)), vc).reshape(bsz, n_ctx, C_Q_W)
    return out_c, out_x


def depthwise_conv_silu(xbc, w, b):
    ch = xbc.shape[-1]
    y = lax.conv_general_dilated(xbc, w[:, None, :].astype(xbc.dtype), window_strides=(1,),
                                 padding=((M_CONV // 2, M_CONV // 2),),
                                 dimension_numbers=('NWC', 'WIO', 'NWC'), feature_group_count=ch)
    return jax.nn.silu(y + b)


def ssd_scan(x, dt, a, bm, cm, h0, with_y):
    bsz, n = x.shape[:2]
    nc = n // M_CHUNK
    rep = M_HEADS // M_GROUPS
    xc = x.astype(jnp.float32).reshape(bsz, nc, M_CHUNK, M_GROUPS, rep, M_HEAD_DIM)
    dtc = dt.reshape(bsz, nc, M_CHUNK, M_GROUPS, rep)
    bc = bm.astype(jnp.float32).reshape(bsz, nc, M_CHUNK, M_GROUPS, M_STATE)
    cc = cm.astype(jnp.float32).reshape(bsz, nc, M_CHUNK, M_GROUPS, M_STATE)
    acs = jnp.cumsum(dtc * a.reshape(M_GROUPS, rep), axis=2)
    decay_end = jnp.exp(acs[:, :, -1:] - acs)
    states = jnp.einsum('bcjgn,bcjgr,bcjgrp->bcgrpn', bc, decay_end * dtc, xc)
    chunk_decay = jnp.exp(acs[:, :, -1])

    def step(h, inp):
        st, dec = inp
        return dec[..., None, None] * h + st, h

    h_final, h_start = lax.scan(step, h0, (jnp.moveaxis(states, 1, 0), jnp.moveaxis(chunk_decay, 1, 0)))
    if not with_y:
        return None, h_final
    h_start = jnp.moveaxis(h_start, 0, 1)
    lower = jnp.tril(jnp.ones((M_CHUNK, M_CHUNK), dtype=bool))
    seg = acs[:, :, :, None] - acs[:, :, None, :]
    lmat = jnp.exp(jnp.where(lower[:, :, None, None], seg, -jnp.inf))
    cb = jnp.einsum('bcign,bcjgn->bcijg', cc, bc)
    w = cb[..., None] * lmat * dtc[:, :, None]
    y = jnp.einsum('bcijgr,bcjgrp->bcigrp', w, xc)
    y = y + jnp.einsum('bcign,bcgrpn->bcigrp', cc, h_start) * jnp.exp(acs)[..., None]
    return y.reshape(bsz, n, M_HEADS, M_HEAD_DIM), h_final


def mixer_d(z_c, xbc_c, dt_c, z_x, xbc_x, dt_x, conv_w, conv_b, dt_bias, a_log, d_skip, norm_g, with_ctx_out):
    a = -jnp.exp(a_log.astype(jnp.float32))

    def prep(xbc, dt_raw):
        bsz, n = xbc.shape[:2]
        xs, bm, cm = split_cols(depthwise_conv_silu(xbc, conv_w, conv_b),
                                (M_INNER, M_GROUPS * M_STATE, M_GROUPS * M_STATE))
        dt = jax.nn.softplus(dt_raw.astype(jnp.float32).reshape(bsz, n, 2, M_HEADS) + dt_bias.astype(jnp.float32))
        return (xs.reshape(bsz, n, M_HEADS, M_HEAD_DIM), bm.reshape(bsz, n, M_GROUPS, M_STATE),
                cm.reshape(bsz, n, M_GROUPS, M_STATE), dt)

    def flip(t):
        return jnp.flip(t, axis=1)

    xs_c, b_c, c_c, dtv_c = prep(xbc_c, dt_c)
    xs_x, b_x, c_x, dtv_x = prep(xbc_x, dt_x)
    bsz = xs_x.shape[0]
    h0 = jnp.zeros((bsz, M_GROUPS, M_HEADS // M_GROUPS, M_HEAD_DIM, M_STATE), jnp.float32)
    yc_f, hc_f = ssd_scan(xs_c, dtv_c[:, :, 0], a[0], b_c, c_c, h0, with_ctx_out)
    yc_b, hc_b = ssd_scan(flip(xs_c), flip(dtv_c[:, :, 1]), a[1], flip(b_c), flip(c_c), h0, with_ctx_out)
    yx_f, _ = ssd_scan(xs_x, dtv_x[:, :, 0], a[0], b_x, c_x, hc_f, True)
    yx_b, _ = ssd_scan(flip(xs_x), flip(dtv_x[:, :, 1]), a[1], flip(b_x), flip(c_x), hc_b, True)

    def readout(y_f, y_b_rev, xs, z):
        y = y_f + flip(y_b_rev) + d_skip.astype(jnp.float32)[:, None] * xs.astype(jnp.float32)
        y = y.reshape(z.shape) * jax.nn.silu(z.astype(jnp.float32))
        return rms_norm(y, norm_g).astype(z.dtype)

    out_c = readout(yc_f, yc_b, xs_c, z_c) if with_ctx_out else None
    return out_c, readout(yx_f, yx_b, xs_x, z_x)


def expert_choice_ffn(h, w_router, w_gate, w_up, w_down):
    bsz, n, _ = h.shape
    cap = CAPACITY_FACTOR * n // N_EXPERTS
    aff = jax.nn.softmax(jnp.einsum('bnd,de->bne', h, w_router, preferred_element_type=jnp.float32), axis=-1)
    gate, idx = lax.top_k(jnp.swapaxes(aff, 1, 2), cap)
    bidx = jnp.arange(bsz)[:, None, None]
    xs = h[bidx, idx]
    hid = jax.nn.silu(jnp.einsum('becd,edf->becf', xs, w_gate)) * jnp.einsum('becd,edf->becf', xs, w_up)
    ye = jnp.einsum('becf,efd->becd', hid, w_down) * gate[..., None].astype(h.dtype)
    return jnp.zeros_like(h).at[bidx, idx].add(ye)


def setup_inputs(seed: int = 0) -> dict:
    key = jax.random.key(seed)
    ks = jax.random.split(key, 35)

    def nrm(i, shape, std=1.0):
        return std * jax.random.normal(ks[i], shape, jnp.float32)

    def unif(i, shape, lo, hi):
        return jax.random.uniform(ks[i], shape, jnp.float32, lo, hi)

    d = D_MODEL
    dt_m = jnp.exp(unif(26, (N_ODD, 2, M_HEADS), math.log(1e-3), math.log(1e-1)))
    return {
        'x': nrm(0, (BATCH, SEQ, d)),
        'c': nrm(1, (BATCH, d)),
        'ctx': nrm(2, (BATCH, CTX_LEN, d)),
        'c_ctx': nrm(3, (d,)),
        'mod_w': nrm(4, (DEPTH, d, 6 * d), 0.5 * d ** -0.5),
        'mod_b': nrm(5, (DEPTH, 6 * d), 0.02),
        'norm_g': 1.0 + nrm(6, (DEPTH, 2, d), 0.02),
        'ev_w_in': nrm(7, (N_EVEN, d, EVEN_IN), d ** -0.5),
        'ev_w_out': nrm(8, (N_EVEN, EVEN_MIX, d), EVEN_MIX ** -0.5),
        'a_q_norm': 1.0 + nrm(9, (N_EVEN, A_HEAD_DIM), 0.02),
        'a_k_norm': 1.0 + nrm(10, (N_EVEN, A_HEAD_DIM), 0.02),
        's5_a_re': -0.5 + nrm(11, (N_EVEN, 2, S5_GROUPS, S5_STATE), 0.01),
        's5_a_im': math.pi * jnp.arange(S5_STATE, dtype=jnp.float32) + nrm(12, (N_EVEN, 2, S5_GROUPS, S5_STATE), 0.01),
        's5_log_dt': unif(13, (N_EVEN, 2, S5_GROUPS), math.log(1e-3), math.log(1e-1)),
        's5_b_re': nrm(14, (N_EVEN, 2, S5_GROUPS, S5_STATE, S5_GROUP), (2 * S5_GROUP) ** -0.5),
        's5_b_im': nrm(15, (N_EVEN, 2, S5_GROUPS, S5_STATE, S5_GROUP), (2 * S5_GROUP) ** -0.5),
        's5_c_re': nrm(16, (N_EVEN, 2, S5_GROUPS, S5_GROUP, S5_STATE), (2 * S5_STATE) ** -0.5),
        's5_c_im': nrm(17, (N_EVEN, 2, S5_GROUPS, S5_GROUP, S5_STATE), (2 * S5_STATE) ** -0.5),
        's5_d': nrm(18, (N_EVEN, S5_W)),
        's5_glu_w': nrm(19, (N_EVEN, S5_W, S5_W), S5_W ** -0.5),
        's5_glu_b': nrm(20, (N_EVEN, S5_W), 0.02),
        'od_w_in': nrm(21, (N_ODD, d, ODD_IN), d ** -0.5),
        'od_w_out': nrm(22, (N_ODD, ODD_MIX, d), ODD_MIX ** -0.5),
        'c_sink': nrm(23, (N_ODD, C_HEADS)),
        'm_conv_w': nrm(24, (N_ODD, M_CONV, M_XBC), M_CONV ** -0.5),
        'm_conv_b': nrm(25, (N_ODD, M_XBC), 0.02),
        'm_dt_bias': dt_m + jnp.log(-jnp.expm1(-dt_m)),
        'm_a_log': jnp.log(unif(27, (N_ODD, 2, M_HEADS), 1.0, 16.0)),
        'm_d': 1.0 + nrm(28, (N_ODD, M_HEADS), 0.1),
        'm_norm_g': 1.0 + nrm(29, (N_ODD, M_INNER), 0.02),
        'moe_router': nrm(30, (DEPTH, d, N_EXPERTS), d ** -0.5),
        'moe_w_gate': nrm(31, (DEPTH, N_EXPERTS, d, EXPERT_FF), d ** -0.5),
        'moe_w_up': nrm(32, (DEPTH, N_EXPERTS, d, EXPERT_FF), d ** -0.5),
        'moe_w_down': nrm(33, (DEPTH, N_EXPERTS, EXPERT_FF, d), EXPERT_FF ** -0.5),
        'final_norm_g': 1.0 + nrm(34, (d,), 0.02),
    }


def reference(x, c, ctx, c_ctx, mod_w, mod_b, norm_g, ev_w_in, ev_w_out, a_q_norm, a_k_norm,
              s5_a_re, s5_a_im, s5_log_dt, s5_b_re, s5_b_im, s5_c_re, s5_c_im, s5_d, s5_glu_w, s5_glu_b,
              od_w_in, od_w_out, c_sink, m_conv_w, m_conv_b, m_dt_bias, m_a_log, m_d, m_norm_g,
              moe_router, moe_w_gate, moe_w_up, moe_w_down, final_norm_g):
    silu_c = jax.nn.silu(c)
    silu_cc = jax.nn.silu(c_ctx)
    for layer in range(DEPTH):
        last = layer == DEPTH - 1
        i = layer // 2
        mx = jnp.split((silu_c @ mod_w[layer] + mod_b[layer])[:, None, :], 6, axis=-1)
        mc = jnp.split(silu_cc @ mod_w[layer] + mod_b[layer], 6, axis=-1)
        hx = rms_norm(x, norm_g[layer, 0]) * (1 + mx[1]) + mx[0]
        hc = rms_norm(ctx, norm_g[layer, 0]) * (1 + mc[1]) + mc[0]
        if layer % 2 == 0:
            w_out = ev_w_out[i]
            pc = split_cols(hc @ ev_w_in[i], EVEN_SPLITS)
            px = split_cols(hx @ ev_w_in[i], EVEN_SPLITS)
            oa_c, oa_x = mixer_a(pc[0], pc[1], pc[2], px[0], px[1], px[2], a_q_norm[i], a_k_norm[i], not last)
            ob_c, ob_x = mixer_b(pc[3], px[3], s5_a_re[i], s5_a_im[i], s5_log_dt[i], s5_b_re[i], s5_b_im[i],
                                 s5_c_re[i], s5_c_im[i], s5_d[i], s5_glu_w[i], s5_glu_b[i], not last)
        else:
            w_out = od_w_out[i]
            pc = split_cols(hc @ od_w_in[i], ODD_SPLITS)
            px = split_cols(hx @ od_w_in[i], ODD_SPLITS)
            oa_c, oa_x = mixer_c(pc[0], pc[1], pc[2], px[0], px[1], px[2], c_sink[i], not last)
            ob_c, ob_x = mixer_d(pc[3], pc[4], pc[5], px[3], px[4], px[5], m_conv_w[i], m_conv_b[i],
                                 m_dt_bias[i], m_a_log[i], m_d[i], m_norm_g[i], not last)
        x = x + mx[2] * (jnp.concatenate([oa_x, ob_x], axis=-1) @ w_out)
        hx = rms_norm(x, norm_g[layer, 1]) * (1 + mx[4]) + mx[3]
        x = x + mx[5] * expert_choice_ffn(hx, moe_router[layer], moe_w_gate[layer], moe_w_up[layer], moe_w_down[layer])
        if not last:
            ctx = ctx + mc[2] * (jnp.concatenate([oa_c, ob_c], axis=-1) @ w_out)
            hc = rms_norm(ctx, norm_g[layer, 1]) * (1 + mc[4]) + mc[3]
            ctx = ctx + mc[5] * expert_choice_ffn(hc, moe_router[layer], moe_w_gate[layer], moe_w_up[layer], moe_w_down[layer])
    return rms_norm(x, final_norm_g)
```

```python
import math
import numpy as np
import concourse.bass as bass
import concourse.mybir as mybir
from concourse.bass_utils import run_bass_kernel_spmd
from contextlib import ExitStack

F32 = mybir.dt.float32
BF16 = mybir.dt.bfloat16
I32 = mybir.dt.int32
U32 = mybir.dt.uint32
ALU = mybir.AluOpType
AF = mybir.ActivationFunctionType
AX = mybir.AxisListType

EPOCH = 24000
NSLOT = 8


class Res:
    __slots__ = ("writers", "readers", "multi", "name", "phase")

    def __init__(self, name="", multi=False):
        self.writers = []
        self.readers = []
        self.multi = multi
        self.name = name
        self.phase = []


class T:
    def __init__(self, t, name, multi=False):
        self.t = t
        self.res = Res(name, multi)
        self.name = name
        self._sub = {}

    def __getitem__(self, k):
        return self.t[k]

    def sub(self, key):
        if key not in self._sub:
            self._sub[key] = Res(f"{self.name}/{key}", False)
        return self._sub[key]


def _res(x):
    return x.res if isinstance(x, T) else x


class Prog:
    ENGS = ("sync", "act", "dve", "pool", "pe")

    def __init__(self, nc):
        self.nc = nc
        self.ops = []
        self.n_comp = {e: 0 for e in self.ENGS}
        self.n_dma = {e: 0 for e in self.ENGS}
        self.stack = ExitStack()
        self.eng_ops = {e: [] for e in self.ENGS}

    def sb(self, name, shape, dtype, multi=False):
        t = self.stack.enter_context(self.nc.sbuf_tensor(name, list(shape), dtype))
        return T(t, name, multi)

    def ps(self, name, shape, dtype):
        t = self.stack.enter_context(self.nc.psum_tensor(name, list(shape), dtype))
        return T(t, name)

    def dram(self, name, shape, dtype, kind="Internal", multi=True):
        t = self.nc.dram_tensor(name, list(shape), dtype, kind=kind)
        return T(t.ap(), name, multi)

    def _record(self, eng, fn, reads, writes, is_dma):
        deps = []
        rs = [_res(r) for r in reads]
        ws = [_res(w) for w in writes]
        for r in rs:
            deps.extend(r.writers)
        op_id = len(self.ops)
        for w in ws:
            if w.multi:
                if w.readers:
                    w.phase = w.readers + w.writers
                    w.writers = []
                    w.readers = []
                deps.extend(w.phase)
            else:
                deps.extend(w.readers)
                deps.extend(w.writers)
        if is_dma:
            k = self.n_dma[eng]
            self.n_dma[eng] += 1
            tok = ("d", eng, k)
        else:
            k = self.n_comp[eng]
            self.n_comp[eng] += 1
            tok = ("c", eng, k)
        op = dict(eng=eng, fn=fn, deps=set(deps), tok=tok, is_dma=is_dma)
        self.ops.append(op)
        self.eng_ops[eng].append(op_id)
        for r in rs:
            r.readers.append(op_id)
        for w in ws:
            if w.multi:
                w.writers.append(op_id)
            else:
                w.writers = [op_id]
                w.readers = []
        return op_id

    def barrier(self):
        snap = (dict(self.n_comp), dict(self.n_dma))
        for e in self.ENGS:
            op = dict(eng=e, fn=None, deps=set(), tok=None, is_dma=False, barrier=snap)
            self.ops.append(op)
            self.eng_ops[e].append(len(self.ops) - 1)

    def op(self, eng, fn, reads=(), writes=()):
        return self._record(eng, fn, reads, writes, False)

    def dma(self, eng, out, in_, reads=(), writes=(), **kw):
        def fn(e):
            return e.dma_start(out=out, in_=in_, **kw)
        return self._record(eng, fn, reads, writes, True)

    def emit(self):
        nc = self.nc
        st = self.stack
        comp_sems = {}
        dma_sems = {}
        for e in self.ENGS:
            ne = (self.n_comp[e] + EPOCH - 1) // EPOCH
            comp_sems[e] = [st.enter_context(nc.semaphore(f"c_{e}_{i}")) for i in range(ne)]
            if self.n_dma[e]:
                dma_sems[e] = [st.enter_context(nc.semaphore(f"d_{e}_{i}")) for i in range(NSLOT)]
        ops = self.ops
        block = st.enter_context(nc.Block())

        def make(ename):
            def body(eng):
                seen_c = {e: -1 for e in self.ENGS}
                seen_d = {}
                my_dma_issued = 0
                for op_id in self.eng_ops[ename]:
                    op = ops[op_id]
                    need_c = {}
                    need_d = {}
                    if op.get("barrier") is not None:
                        sc, sd = op["barrier"]
                        for e2 in self.ENGS:
                            k2 = sc[e2] - 1
                            if k2 > seen_c[e2] and not (ename == e2):
                                eng.wait_ge(comp_sems[e2][k2 // EPOCH], (k2 % EPOCH) + 1)
                                seen_c[e2] = k2
                            if k2 >= 0 and ename == e2 and k2 > seen_c[e2]:
                                eng.wait_ge(comp_sems[e2][k2 // EPOCH], (k2 % EPOCH) + 1)
                                seen_c[e2] = k2
                            nd2 = sd[e2]
                            for slot in range(min(NSLOT, nd2)):
                                k3 = ((nd2 - 1 - slot) // NSLOT) * NSLOT + slot
                                if k3 > seen_d.get((e2, slot), -1):
                                    eng.wait_ge(dma_sems[e2][slot], 16 * (k3 // NSLOT + 1))
                                    seen_d[(e2, slot)] = k3
                        continue
                    for d in op["deps"]:
                        kind, e2, k2 = ops[d]["tok"]
                        if kind == "c":
                            if ename == "pe" and e2 == "pe":
                                continue
                            if k2 > seen_c[e2] and k2 > need_c.get(e2, -1):
                                need_c[e2] = k2
                        else:
                            key = (e2, k2 % NSLOT)
                            if k2 > seen_d.get(key, -1) and k2 > need_d.get(key, -1):
                                need_d[key] = k2
                    if op["is_dma"]:
                        k = op["tok"][2]
                        if k >= NSLOT:
                            key = (ename, k % NSLOT)
                            kk = k - NSLOT
                            if kk > seen_d.get(key, -1) and kk > need_d.get(key, -1):
                                need_d[key] = kk
                    for e2, k2 in need_c.items():
                        eng.wait_ge(comp_sems[e2][k2 // EPOCH], (k2 % EPOCH) + 1)
                        seen_c[e2] = k2
                    for (e2, slot), k2 in need_d.items():
                        eng.wait_ge(dma_sems[e2][slot], 16 * (k2 // NSLOT + 1))
                        seen_d[(e2, slot)] = k2
                    ins = op["fn"](eng)
                    kind, _, k = op["tok"]
                    if kind == "c":
                        ins.then_inc(comp_sems[ename][k // EPOCH], 1)
                    else:
                        ins.then_inc(dma_sems[ename][k % NSLOT], 16)
                nd = self.n_dma[ename]
                for slot in range(min(NSLOT, nd)):
                    cnt = (nd - 1 - slot) // NSLOT + 1
                    eng.wait_ge(dma_sems[ename][slot], 16 * cnt)
            return body

        block.sync(make("sync"))
        block.scalar(make("act"))
        block.vector(make("dve"))
        block.gpsimd(make("pool"))
        block.tensor(make("pe"))
        st.close()

D = 2048
NT = 18
TOK = 2304
EPS = 1e-6
TWO_PI = 2.0 * math.pi


class Arena:
    def __init__(self, P, nwords=49152):
        self.P = P
        self.base = P.sb("arena", [128, nwords], F32)
        self.off = 0
        self.cap = nwords * 4

    def alloc(self, name, shape, dtype, parts=128):
        esz = 2 if dtype == BF16 else 4
        n = 1
        for s in shape:
            n *= s
        nbytes = (n * esz + 31) // 32 * 32
        assert self.off + nbytes <= self.cap, (name, self.off, nbytes)
        ap = self.base.t[0:parts, self.off // 4:(self.off + nbytes) // 4]
        if dtype != F32:
            ap = ap.bitcast(dtype)
        ap = ap[:, 0:n]
        if len(shape) == 2:
            ap = ap.rearrange("p (a b) -> p a b", a=shape[0])
        elif len(shape) == 3:
            ap = ap.rearrange("p (a b c) -> p a b c", a=shape[0], b=shape[1])
        self.off += nbytes
        return T(ap, name)

    def mark(self):
        return self.off

    def release(self, m):
        self.off = m
        self.P.barrier()


def build_program(n_layers=4, dbg=None, unit=None, upto=99):
    nc = bass.Bass("TRN2", target_bir_lowering=False)
    P = Prog(nc)
    A = Arena(P)
    PS = P.ps("psall", [128, 4096], F32)
    PB = [Res(f"bank{b}") for b in range(8)]

    def pf(b, n=1):
        return PS.t[:, b * 512:(b + n) * 512]

    def pbf(b, n=1):
        return PS.t[:, b * 512:(b + n) * 512].bitcast(BF16)

    def inp(name, shape):
        return nc.dram_tensor(name, list(shape), F32, kind="ExternalInput").ap()

    x_in = inp("x_in", [2048, D] if unit is None else [1, 1]); ctx_in = inp("ctx_in", [256, D]); cT_in = inp("cT_in", [128, 32])
    mod_w = inp("mod_w", [4, D, 6 * D] if unit is None else [1, 1]); mod_b = inp("mod_b", [4, 6 * D]); norm_g = inp("norm_g", [8, D])
    ev_w_in = inp("ev_w_in", [2, D, 2560] if unit is None else [1, 1]); ev_w_out = inp("ev_w_out", [2, D, D] if unit is None else [1, 1])
    a_q_norm = inp("a_q_norm", [2, 128]); a_k_norm = inp("a_k_norm", [2, 128])
    s5_par = inp("s5_par", [2, 2, 128, 96])
    s5_bblk = inp("s5_bblk", [2, 2, 2, 32, 128, 128] if unit is None else [1, 1])
    s5_cblk = inp("s5_cblk", [2, 2, 2, 128, 32, 16] if unit is None else [1, 1])
    s5_d = inp("s5_d", [2, 1024]); s5_glu_w = inp("s5_glu_w", [2, 1024, 1024] if unit is None else [1, 1]); s5_glu_b = inp("s5_glu_b", [2, 1024])
    od_w_in = inp("od_w_in", [2, D, 3872] if unit is None else [1, 1]); od_w_out = inp("od_w_out", [2, D, D] if unit is None else [1, 1])
    c_sink = inp("c_sink", [2, 16]); m_conv_w = inp("m_conv_w", [2, 3, 1536]); m_conv_b = inp("m_conv_b", [2, 1536])
    m_dt_bias = inp("m_dt_bias", [2, 32]); m_a_log = inp("m_a_log", [2, 32]); m_d = inp("m_d", [2, 16]); m_norm_g = inp("m_norm_g", [2, 1024])
    moe_router = inp("moe_router", [4, D, 16] if unit is None else [1, 1]); moe_w_gate = inp("moe_w_gate", [4, 16, D, 1024] if unit is None else [1, 1])
    moe_w_up = inp("moe_w_up", [4, 16, D, 1024] if unit is None else [1, 1]); moe_w_down = inp("moe_w_down", [4, 16, 1024, D] if unit is None else [1, 1])
    final_norm_g = inp("final_norm_g", [1, D])
    k_ident = inp("k_ident", [128, 128]); k_iota = inp("k_iota", [128, 256]); k_iotap = inp("k_iotap", [128, 2])
    k_spos = inp("k_spos", [2, 128, TOK]); k_rope = inp("k_rope", [4, 2048, 64])
    k_maskw = inp("k_maskw", [128, 384]); k_tri = inp("k_tri", [4, 128, 128])
    y_out = nc.dram_tensor("y_out", [2048, D], F32, kind="ExternalOutput").ap()

    X = P.dram("X", [TOK, D], F32)
    MODV = P.dram("MODV", [2, 6 * D], F32)
    HT = P.dram("HT", [D, TOK], BF16)
    HTOK = P.dram("HTOK", [TOK, D], BF16)
    PROJ = P.dram("PROJ", [TOK, 3872], F32)
    MIX = P.dram("MIX", [TOK, D], BF16)
    MIXT = P.dram("MIXT", [D, TOK], BF16)
    ROUT = P.dram("ROUT", [3, 16, TOK], F32)
    YE = P.dram("YE", [16, 288, D], BF16)
    GT5 = P.dram("GT5", [TOK, 1024], BF16)
    GT5T = P.dram("GT5T", [1024, TOK], BF16)
    YF = P.dram("YF", [TOK, 1024], F32)
    XC = P.dram("XC", [TOK, 1536], BF16)
    DTD = P.dram("DTD", [TOK, 64], F32)
    SZ = P.dram("SZ", [TOK, 1024], F32)
    YB = P.dram("YB", [TOK, 1024], F32)

    def rows(i):
        return slice(i * 128, (i + 1) * 128)

    def mm(out, lhsT, rhs, start, stop, reads, writes):
        P.op("pe", lambda e: e.matmul(out, lhsT=lhsT, rhs=rhs, start=start, stop=stop), reads, writes)

    def tr(out, in_, ident, reads, writes):
        P.op("pe", lambda e: e.transpose(out, in_, ident), reads, writes)

    def act(out, in_, func, reads, writes, bias=None, scale=None, accum=None, eng="act"):
        kw = {}
        if bias is not None:
            kw["bias"] = bias
        if scale is not None:
            kw["scale"] = scale
        if accum is not None:
            kw["accum_out"] = accum
        P.op("act", lambda e: e.activation(out=out, in_=in_, func=func, **kw), reads, writes)

    def tt(eng, out, a, b, op, reads, writes):
        P.op(eng, lambda e: e.tensor_tensor(out=out, in0=a, in1=b, op=op), reads, writes)

    def ts(eng, out, a, s1, s2, op0, op1, reads, writes, accum=None):
        kw = {}
        if accum is not None:
            kw["accum_out"] = accum
        if op1 is None:
            P.op(eng, lambda e: e.tensor_scalar(out=out, in0=a, scalar1=s1, scalar2=None, op0=op0, **kw), reads, writes)
        else:
            P.op(eng, lambda e: e.tensor_scalar(out=out, in0=a, scalar1=s1, scalar2=s2, op0=op0, op1=op1, **kw), reads, writes)

    def stt(eng, out, a, s, b, op0, op1, reads, writes):
        P.op(eng, lambda e: e.scalar_tensor_tensor(out=out, in0=a, scalar=s, in1=b, op0=op0, op1=op1), reads, writes)

    def cp(eng, out, in_, reads, writes):
        if eng == "act":
            P.op("act", lambda e: e.copy(out=out, in_=in_), reads, writes)
        else:
            P.op(eng, lambda e: e.tensor_copy(out=out, in_=in_), reads, writes)

    def recip(out, in_, reads, writes):
        P.op("dve", lambda e: e.reciprocal(out=out, in_=in_), reads, writes)

    def gen(eng, name, reads, writes, **kw):
        P.op(eng, lambda e: getattr(e, name)(**kw), reads, writes)

    def bcast(ap, n=128):
        return ap.partition_broadcast(n)

    ident_bf = A.alloc("ident_bf", [128], BF16)
    ident_f = A.alloc("ident_f", [128], F32)
    cTs = A.alloc("cTs", [16, 2], F32)
    iotaS = A.alloc("iotaS", [256], F32)
    iotaP = A.alloc("iotaP", [2], F32)
    P.dma("pool", ident_bf[:], k_ident, writes=[ident_bf])
    P.dma("sync", ident_f[:], k_ident, writes=[ident_f])
    P.dma("sync", iotaS[:], k_iota, writes=[iotaS])
    P.dma("sync", iotaP[:], k_iotap, writes=[iotaP])
    P.dma("sync", cTs[:].rearrange("p a b -> p (a b)"), cT_in, writes=[cTs])
    act(cTs[:], cTs[:], AF.Silu, [cTs], [cTs])
    if unit is None:
        P.dma("sync", X[0:256, :], ctx_in, writes=[X.sub(0), X.sub(1)])
        for i in range(16):
            P.dma("sync", X[rows(i + 2), :], x_in[rows(i), :], writes=[X.sub(i + 2)])

    def stage_mod(l):
        m = A.mark()
        wbufs = [A.alloc(f"modw{i}", [2048], F32) for i in range(3)]
        bb = A.alloc("modbb", [2048], F32, parts=2)
        ot = A.alloc("modo", [2048], F32, parts=2)
        n = 0
        for cg in range(6):
            c0 = cg * 2048
            for k in range(16):
                wb = wbufs[n % 3]; n += 1
                P.dma("sync", wb[:], mod_w[l, k * 128:(k + 1) * 128, c0:c0 + 2048], writes=[wb])
                for q in range(4):
                    mm(pf(q)[0:2, :], cTs[:, k, :], wb[:, q * 512:(q + 1) * 512], k == 0, k == 15, [wb, cTs], [PB[q]])
            P.dma("sync", bb[:], bcast(mod_b[l:l + 1, c0:c0 + 2048], 2), writes=[bb])
            tt("dve", ot[:], pf(0, 4)[0:2, :], bb[:], ALU.add, [PB[0], PB[1], PB[2], PB[3], bb], [ot])
            P.dma("pool", MODV[:, c0:c0 + 2048], ot[:], reads=[ot], writes=[MODV])
        A.release(m)

    def stage_norm(g_row, sh_idx, sc_idx, dstT, dst_tok, tiles=range(NT), final_out=None):
        m = A.mark()
        G = [A.alloc(f"nG{s}", [2048], F32) for s in range(2)]
        S = [A.alloc(f"nS{s}", [2048], F32) for s in range(2)]
        gt = A.alloc("ngt", [2048], F32)
        tmp = A.alloc("ntmp", [2048], F32)
        P.dma("sync", gt[:], bcast(g_row), writes=[gt])
        if final_out is None:
            for s in range(2):
                r = 1 if s == 0 else 0
                P.dma("sync", tmp[:], bcast(MODV[r:r + 1, sc_idx * 2048:(sc_idx + 1) * 2048]), reads=[MODV], writes=[tmp])
                stt("dve", G[s][:], tmp[:], 1.0, gt[:], ALU.add, ALU.mult, [tmp, gt], [G[s]])
                P.dma("sync", S[s][:], bcast(MODV[r:r + 1, sh_idx * 2048:(sh_idx + 1) * 2048]), reads=[MODV], writes=[S[s]])
        xts = [A.alloc(f"nx{i}", [2048], F32) for i in range(2)]
        junk = A.alloc("njunk", [2048], F32)
        hn = A.alloc("nhn", [2048], F32)
        hbs = [A.alloc(f"nhb{i}", [2048], BF16) for i in range(2)]
        hTs = [A.alloc(f"nhT{i}", [16, 128], BF16) for i in range(2)]
        sss = [A.alloc(f"nss{i}", [2], F32) for i in range(2)]
        dstT_v = dstT.t.rearrange("(k p) t -> p k t", p=128) if dstT is not None else None
        for n, i in enumerate(tiles):
            s = 0 if i < 2 else 1
            xt = xts[n % 2]; hb = hbs[n % 2]; hT = hTs[n % 2]; ss = sss[n % 2]
            P.dma("sync", xt[:], X[rows(i), :], reads=[X.sub(i)], writes=[xt])
            act(junk[:], xt[:], AF.Square, [xt], [junk, ss], accum=ss[:, 0:1])
            ts("dve", ss[:, 1:2], ss[:, 0:1], 1.0 / D, EPS, ALU.mult, ALU.add, [ss], [ss])
            act(ss[:, 1:2], ss[:, 1:2], AF.Sqrt, [ss], [ss])
            recip(ss[:, 1:2], ss[:, 1:2], [ss], [ss])
            if final_out is not None:
                stt("dve", hn[:], xt[:], ss[:, 1:2], gt[:], ALU.mult, ALU.mult, [xt, ss, gt], [hn])
                P.dma("pool", final_out[rows(i - 2), :], hn[:], reads=[hn])
                continue
            stt("dve", hn[:], xt[:], ss[:, 1:2], G[s][:], ALU.mult, ALU.mult, [xt, ss, G[s]], [hn])
            tt("pool", hb[:], hn[:], S[s][:], ALU.add, [hn, S[s]], [hb])
            if dst_tok is not None:
                P.dma("pool", dst_tok[rows(i), :], hb[:], reads=[hb], writes=[dst_tok.sub(i)])
            for q in range(2):
                bk = 6 + q
                for jj in range(8):
                    k = q * 8 + jj
                    tr(pbf(bk)[:, jj * 128:(jj + 1) * 128], hb[:, k * 128:(k + 1) * 128], ident_bf[:], [hb, ident_bf], [PB[bk]])
                cp("act" if q == 0 else "dve", hT[:, q * 8:(q + 1) * 8, :], pbf(bk).rearrange("p (a b) -> p a b", a=8), [PB[bk]], [hT])
            P.dma("pool", dstT_v[:, :, rows(i)], hT[:], reads=[hT], writes=[dstT.sub(i)])
        A.release(m)

    def stage_transpose(SRC, DST, ncol):
        m = A.mark()
        kt = ncol // 128
        tbs = [A.alloc(f"tb{i}", [ncol], BF16) for i in range(2)]
        tTs = [A.alloc(f"tT{i}", [kt, 128], BF16) for i in range(2)]
        dv = DST.t.rearrange("(k p) t -> p k t", p=128)
        for i in range(NT):
            tb = tbs[i % 2]; tT = tTs[i % 2]
            P.dma("sync", tb[:], SRC[rows(i), 0:ncol], reads=[SRC], writes=[tb])
            for q in range(kt // 8):
                bk = 6 + (q % 2)
                for jj in range(8):
                    k = q * 8 + jj
                    tr(pbf(bk)[:, jj * 128:(jj + 1) * 128], tb[:, k * 128:(k + 1) * 128], ident_bf[:], [tb, ident_bf], [PB[bk]])
                cp("act" if q % 2 == 0 else "dve", tT[:, q * 8:(q + 1) * 8, :], pbf(bk).rearrange("p (a b) -> p a b", a=8), [PB[bk]], [tT])
            P.dma("pool", dv[:, 0:kt, rows(i)], tT[:], reads=[tT], writes=[DST.sub(i)])
        A.release(m)

    def stage_linear(srcT, KT, W, N, epi_factory, GW=1024):
        m = A.mark()
        wbs = [A.alloc(f"lw{i}", [KT, GW], BF16) for i in range(2)]
        hts = [A.alloc(f"lh{i}", [KT, 128], BF16) for i in range(3)]
        epi = epi_factory()
        sv = srcT.t.rearrange("(k p) t -> p k t", p=128)
        ng = (N + GW - 1) // GW
        cnt = 0
        for gi in range(ng):
            c0 = gi * GW
            gw = min(GW, N - c0)
            wb = wbs[gi % 2]
            for kq in range(0, KT, 4):
                P.dma("pool", wb[:, kq:kq + 4, 0:gw], W[kq * 128:(kq + 4) * 128, c0:c0 + gw].rearrange("(k p) n -> p k n", p=128), writes=[wb])
            for i in range(NT):
                ht = hts[cnt % 3]
                pbase = 0 if cnt % 2 == 0 else 2
                cnt += 1
                P.dma("sync", ht[:], sv[:, 0:KT, rows(i)], reads=[srcT.sub(i)], writes=[ht])
                nch = (gw + 511) // 512
                for ch in range(nch):
                    cw = min(512, gw - ch * 512)
                    for k in range(KT):
                        mm(pf(pbase + ch)[:, 0:cw], ht[:, k, :], wb[:, k, ch * 512:ch * 512 + cw], k == 0, k == KT - 1, [ht, wb], [PB[pbase + ch]])
                epi(i, c0, gw, pf(pbase, 2)[:, 0:gw], [PB[pbase], PB[pbase + 1]])
        A.release(m)

    def epi_store(DST, col_off=0):
        def factory():
            obs = [A.alloc(f"eo{i}", [1024], F32) for i in range(2)]
            st = {"n": 0}

            def epi(i, c0, gw, psap, pres):
                ob = obs[st["n"] % 2]; st["n"] += 1
                cp("act", ob[:, 0:gw], psap, pres, [ob])
                P.dma("pool", DST[rows(i), col_off + c0:col_off + c0 + gw], ob[:, 0:gw], reads=[ob], writes=[DST])
            return epi
        return factory

    def load_gate(gate_idx):
        GT = [A.alloc(f"eG{s}", [2048], F32) for s in range(2)]
        for s in range(2):
            r = 1 if s == 0 else 0
            P.dma("sync", GT[s][:], bcast(MODV[r:r + 1, gate_idx * 2048:(gate_idx + 1) * 2048]), reads=[MODV], writes=[GT[s]])
        return GT

    def epi_resid(gate_idx):
        def factory():
            GT = load_gate(gate_idx)
            xbs = [A.alloc(f"ex{i}", [1024], F32) for i in range(2)]
            tbs = [A.alloc(f"et{i}", [1024], F32) for i in range(2)]
            st = {"n": 0}

            def epi(i, c0, gw, psap, pres):
                s = 0 if i < 2 else 1
                xb = xbs[st["n"] % 2]; tb = tbs[st["n"] % 2]; st["n"] += 1
                P.dma("sync", xb[:, 0:gw], X[rows(i), c0:c0 + gw], reads=[X.sub(i)], writes=[xb])
                tt("dve", tb[:, 0:gw], psap, GT[s][:, c0:c0 + gw], ALU.mult, pres + [GT[s]], [tb])
                tt("pool", xb[:, 0:gw], xb[:, 0:gw], tb[:, 0:gw], ALU.add, [xb, tb], [xb])
                P.dma("pool", X[rows(i), c0:c0 + gw], xb[:, 0:gw], reads=[xb], writes=[X.sub(i)])
            return epi
        return factory

    STREAMS = [(0, 2, 32), (2, 16, 256)]
    YE_OFF = [0, 32]

    def stage_moe(l):
        m = A.mark()
        wr = A.alloc("wr", [16, 16], BF16)
        P.dma("pool", wr[:], moe_router[l].rearrange("(k p) e -> p k e", p=128), writes=[wr])
        hts = [A.alloc(f"rh{i}", [16, 128], BF16) for i in range(2)]
        aff = A.alloc("aff", [16], F32)
        sm = A.alloc("rsm", [4], F32)
        affT = A.alloc("affT", [TOK], F32, parts=16)
        work = A.alloc("rwork", [2048], F32, parts=16)
        m8 = A.alloc("m8", [8], F32, parts=16)
        maskT = A.alloc("maskT", [TOK], F32, parts=16)
        gateT = A.alloc("gateT", [TOK], F32, parts=16)
        posT = A.alloc("posT", [TOK], F32, parts=16)
        ones = A.alloc("rones", [2048], F32, parts=16)
        P.op("dve", lambda e: e.memset(ones[:], 1.0), [], [ones])
        sv = HT.t.rearrange("(k p) t -> p k t", p=128)
        for i in range(NT):
            ht = hts[i % 2]
            P.dma("sync", ht[:], sv[:, :, rows(i)], reads=[HT.sub(i)], writes=[ht])
            for k in range(16):
                mm(pf(4)[:, 0:16], ht[:, k, :], wr[:, k, :], k == 0, k == 15, [ht, wr], [PB[4]])
            P.op("dve", lambda e: e.reduce_max(out=sm[:, 0:1], in_=pf(4)[:, 0:16], axis=AX.X), [PB[4]], [sm])
            ts("dve", sm[:, 1:2], sm[:, 0:1], -1.0, None, ALU.mult, None, [sm], [sm])
            act(aff[:], pf(4)[:, 0:16], AF.Exp, [PB[4], sm], [aff, sm], bias=sm[:, 1:2], accum=sm[:, 2:3])
            recip(sm[:, 3:4], sm[:, 2:3], [sm], [sm])
            ts("dve", aff[:], aff[:], sm[:, 3:4], None, ALU.mult, None, [aff, sm], [aff])
            tr(pf(5)[0:16, 0:128], aff[:, 0:16], ident_f[:], [aff, ident_f], [PB[5]])
            cp("act", affT[:, rows(i)], pf(5)[0:16, 0:128], [PB[5]], [affT])
        for (t0, ntl, cap) in STREAMS:
            c0 = t0 * 128; Tn = ntl * 128
            cp("dve", work[:, 0:Tn], affT[:, c0:c0 + Tn], [affT], [work])
            for r in range(cap // 8):
                gen("dve", "max", [work], [m8], out=m8[:], in_=work[:, 0:Tn])
                if r < cap // 8 - 1:
                    gen("dve", "match_replace", [work, m8], [work], out=work[:, 0:Tn], in_to_replace=m8[:], in_values=work[:, 0:Tn], imm_value=-1.0)
            ts("dve", maskT[:, c0:c0 + Tn], affT[:, c0:c0 + Tn], m8[:, 7:8], None, ALU.is_ge, None, [affT, m8], [maskT])
            tt("dve", gateT[:, c0:c0 + Tn], affT[:, c0:c0 + Tn], maskT[:, c0:c0 + Tn], ALU.mult, [affT, maskT], [gateT])
            gen("dve", "tensor_tensor_scan", [ones, maskT], [posT], out=posT[:, c0:c0 + Tn], data0=ones[:, 0:Tn], data1=maskT[:, c0:c0 + Tn], initial=0.0, op0=ALU.mult, op1=ALU.add)
            ts("dve", posT[:, c0:c0 + Tn], posT[:, c0:c0 + Tn], -1.0, None, ALU.add, None, [posT], [posT])
        P.dma("pool", ROUT[0], posT[:], reads=[posT], writes=[ROUT])
        P.dma("pool", ROUT[1], gateT[:], reads=[gateT], writes=[ROUT])
        m_keep = A.mark()
        A.off = m
        A.P.barrier()
        postok = A.alloc("postok", [NT, 16], F32)
        masktok = A.alloc("masktok", [NT, 16], F32)
        for i in range(NT):
            tr(pf(5)[:, 0:16], posT[:, rows(i)], ident_f[0:16, 0:16], [posT, ident_f], [PB[5]])
            cp("act", postok[:, i, :], pf(5)[:, 0:16], [PB[5]], [postok])
            tr(pf(4)[:, 0:16], maskT[:, rows(i)], ident_f[0:16, 0:16], [maskT, ident_f], [PB[4]])
            cp("dve", masktok[:, i, :], pf(4)[:, 0:16], [PB[4]], [masktok])
        A.P.barrier()
        hkb = [A.alloc(f"hk{i}", [16, 512], BF16) for i in range(2)]
        ring = [A.alloc(f"wring{i}", [8192], BF16) for i in range(6)]
        sel = A.alloc("sel", [16, 256], BF16)
        xsT = A.alloc("xsT", [16, 256], BF16)
        hidT = A.alloc("hidT", [8, 256], BF16)
        stmp = A.alloc("stmp", [256], F32)
        yebs = [A.alloc(f"yeb{i}", [2048], BF16) for i in range(2)]
        rn = 0
        yn = 0
        for e in range(16):
            wg = []; wu = []; wd = []
            for h2 in range(2):
                b = ring[rn % 6]; rn += 1
                P.dma("pool", b[:].rearrange("p (k n) -> p k n", k=16), moe_w_gate[l, e, :, h2 * 512:(h2 + 1) * 512].rearrange("(k p) n -> p k n", p=128), writes=[b])
                wg.append(b)
                b = ring[rn % 6]; rn += 1
                P.dma("pool", b[:].rearrange("p (k n) -> p k n", k=16), moe_w_up[l, e, :, h2 * 512:(h2 + 1) * 512].rearrange("(k p) n -> p k n", p=128), writes=[b])
                wu.append(b)
            for h2 in range(2):
                b = ring[rn % 6]; rn += 1
                P.dma("pool", b[:].rearrange("p (k n) -> p k n", k=4), moe_w_down[l, e, h2 * 512:(h2 + 1) * 512, :].rearrange("(k p) n -> p k n", p=128), writes=[b])
                wd.append(b)
            for si, (t0, ntl, cap) in enumerate(STREAMS):
                S_ = cap
                SP = min(S_, 128)
                SH = (S_ + 127) // 128
                for tl in range(ntl):
                    ts("dve", sel[:, tl, 0:S_], iotaS[:, 0:S_], postok[:, t0 + tl, e:e + 1], masktok[:, t0 + tl, e:e + 1], ALU.is_equal, ALU.mult, [iotaS, postok, masktok], [sel])
                for dq in range(4):
                    hk = hkb[dq % 2]
                    P.dma("sync", hk[:, 0:ntl, :], HTOK[t0 * 128:(t0 + ntl) * 128, dq * 512:(dq + 1) * 512].rearrange("(n p) c -> p n c", p=128),
                          reads=[HTOK.sub(t0 + j) for j in range(ntl)], writes=[hk])
                    for dl in range(4):
                        dt_ = dq * 4 + dl
                        bk = dl % 2
                        for tl in range(ntl):
                            mm(pf(bk)[:, 0:S_], hk[:, tl, dl * 128:(dl + 1) * 128], sel[:, tl, 0:S_], tl == 0, tl == ntl - 1, [hk, sel], [PB[bk]])
                        cp("act" if dl % 2 == 0 else "dve", xsT[:, dt_, 0:S_], pf(bk)[:, 0:S_], [PB[bk]], [xsT])
                for f in range(8):
                    h2 = f // 4; fl = f % 4
                    wgv = wg[h2][:].rearrange("p (k n) -> p k n", k=16)
                    wuv = wu[h2][:].rearrange("p (k n) -> p k n", k=16)
                    for k in range(16):
                        mm(pf(2)[:, 0:S_], wgv[:, k, fl * 128:(fl + 1) * 128], xsT[:, k, 0:S_], k == 0, k == 15, [wg[h2], xsT], [PB[2]])
                    for k in range(16):
                        mm(pf(3)[:, 0:S_], wuv[:, k, fl * 128:(fl + 1) * 128], xsT[:, k, 0:S_], k == 0, k == 15, [wu[h2], xsT], [PB[3]])
                    act(stmp[:, 0:S_], pf(2)[:, 0:S_], AF.Silu, [PB[2]], [stmp])
                    tt("dve", hidT[:, f, 0:S_], stmp[:, 0:S_], pf(3)[:, 0:S_], ALU.mult, [stmp, PB[3]], [hidT])
                for sh in range(SH):
                    yeb = yebs[yn % 2]; yn += 1
                    for dc in range(4):
                        bk = 4 + dc % 2
                        for f in range(8):
                            wdv = wd[f // 4][:].rearrange("p (k n) -> p k n", k=4)
                            mm(pf(bk)[0:SP, :], hidT[:, f, sh * 128:sh * 128 + SP], wdv[:, f % 4, dc * 512:(dc + 1) * 512], f == 0, f == 7, [hidT, wd[f // 4]], [PB[bk]])
                        cp("act" if dc % 2 == 0 else "dve", yeb[0:SP, dc * 512:(dc + 1) * 512], pf(bk)[0:SP, :], [PB[bk]], [yeb])
                    r0 = YE_OFF[si] + sh * 128
                    P.dma("sync", YE[e, r0:r0 + SP, :], yeb[0:SP, :], reads=[yeb], writes=[YE])
        A.release(m)
        m = A.mark()
        GT = load_gate(5)
        for si, (t0, ntl, cap) in enumerate(STREAMS):
            m2 = A.mark()
            S_ = cap
            SP = min(S_, 128)
            SH = (S_ + 127) // 128
            yall = A.alloc("yall", [16 * SH, 2048], BF16, parts=SP)
            for e in range(16):
                P.dma("sync", yall[:, e * SH:(e + 1) * SH, :], YE[e, YE_OFF[si]:YE_OFF[si] + S_, :].rearrange("(h p) d -> p h d", p=SP), reads=[YE], writes=[yall])
            posb = A.alloc("posb", [16, 128], F32)
            gateb = A.alloc("gateb", [16, 128], F32)
            tmpf = A.alloc("tmpf", [16, 128], F32)
            stg = [A.alloc(f"stg{h}", [16, 128], BF16) for h in range(SH)]
            xbs = [A.alloc(f"sx{i}", [512], F32) for i in range(2)]
            tbs = [A.alloc(f"st{i}", [512], F32) for i in range(2)]
            n = 0
            for tl in range(ntl):
                i = t0 + tl
                s = 0 if i < 2 else 1
                P.dma("sync", posb[0:SP], bcast(ROUT[0:1, :, rows(i)], SP), reads=[ROUT], writes=[posb])
                P.dma("sync", gateb[0:SP], bcast(ROUT[1:2, :, rows(i)], SP), reads=[ROUT], writes=[gateb])
                for sh in range(SH):
                    ts("dve", tmpf[0:SP], posb[0:SP], iotaP[0:SP, sh:sh + 1], None, ALU.is_equal, None, [posb, iotaP], [tmpf])
                    tt("dve", stg[sh][0:SP], tmpf[0:SP], gateb[0:SP], ALU.mult, [tmpf, gateb], [stg[sh]])
                for dc in range(4):
                    bk = dc % 2
                    tot = 16 * SH
                    q = 0
                    for e in range(16):
                        for sh in range(SH):
                            mm(pf(bk)[:, :], stg[sh][0:SP, e, :], yall[0:SP, e * SH + sh, dc * 512:(dc + 1) * 512], q == 0, q == tot - 1, [stg[sh], yall], [PB[bk]])
                            q += 1
                    xb = xbs[n % 2]; tb = tbs[n % 2]; n += 1
                    P.dma("sync", xb[:], X[rows(i), dc * 512:(dc + 1) * 512], reads=[X.sub(i)], writes=[xb])
                    tt("dve", tb[:], pf(bk), GT[s][:, dc * 512:(dc + 1) * 512], ALU.mult, [PB[bk], GT[s]], [tb])
                    tt("pool", xb[:], xb[:], tb[:], ALU.add, [xb, tb], [xb])
                    P.dma("pool", X[rows(i), dc * 512:(dc + 1) * 512], xb[:], reads=[xb], writes=[X.sub(i)])
            A.release(m2)
        A.release(m)

    def stage_attn_a(li):
        m = A.mark()
        qT = A.alloc("qT", [8, TOK], BF16)
        kT = A.alloc("kT", [2, TOK], BF16)
        vA = A.alloc("vA", [NT, 256], BF16)
        gq = A.alloc("gq", [128], F32); gk = A.alloc("gk", [128], F32)
        P.dma("sync", gq[:], bcast(a_q_norm[li:li + 1, :]), writes=[gq])
        P.dma("sync", gk[:], bcast(a_k_norm[li:li + 1, :]), writes=[gk])
        m1 = A.mark()
        qks = [A.alloc(f"qk{i}", [10, 128], F32) for i in range(2)]
        sq = A.alloc("qsq", [10, 128], F32)
        qn = A.alloc("qn", [10, 128], F32)
        qb = A.alloc("qb", [10, 128], BF16)
        ssq = A.alloc("ssq", [10], F32)
        cs = [A.alloc(f"cs{i}", [2, 64], F32) for i in range(2)]
        t1 = A.alloc("rt1", [10, 64], F32); t2 = A.alloc("rt2", [10, 64], F32)
        vf = [A.alloc(f"vf{i}", [256], F32) for i in range(2)]
        for i in range(NT):
            qk = qks[i % 2]
            P.dma("sync", qk[:].rearrange("p a b -> p (a b)"), PROJ[rows(i), 0:1280], reads=[PROJ], writes=[qk])
            P.dma("sync", vf[i % 2][:], PROJ[rows(i), 1280:1536], reads=[PROJ], writes=[vf[i % 2]])
            cp("pool", vA[:, i, :], vf[i % 2][:], [vf[i % 2]], [vA])
            tt("pool", sq[:], qk[:], qk[:], ALU.mult, [qk], [sq])
            P.op("dve", lambda e: e.tensor_reduce(out=ssq[:], in_=sq[:], axis=AX.X, op=ALU.add), [sq], [ssq])
            ts("dve", ssq[:], ssq[:], 1.0 / 128, EPS, ALU.mult, ALU.add, [ssq], [ssq])
            act(ssq[:], ssq[:], AF.Sqrt, [ssq], [ssq])
            recip(ssq[:], ssq[:], [ssq], [ssq])
            tt("dve", qn[:], qk[:], ssq[:].unsqueeze(2).to_broadcast([128, 10, 128]), ALU.mult, [qk, ssq], [qn])
            tt("dve", qn[:, 0:8, :], qn[:, 0:8, :], gq[:].unsqueeze(1).to_broadcast([128, 8, 128]), ALU.mult, [qn, gq], [qn])
            tt("dve", qn[:, 8:10, :], qn[:, 8:10, :], gk[:].unsqueeze(1).to_broadcast([128, 2, 128]), ALU.mult, [qn, gk], [qn])
            if i >= 2:
                c = cs[i % 2]
                P.dma("sync", c[:, 0, :], k_rope[0, rows(i - 2), :], writes=[c])
                P.dma("sync", c[:, 1, :], k_rope[1, rows(i - 2), :], writes=[c])
                cb = c[:, 0, :].unsqueeze(1).to_broadcast([128, 10, 64])
                sb = c[:, 1, :].unsqueeze(1).to_broadcast([128, 10, 64])
                x1 = qn[:, :, 0:64]; x2 = qn[:, :, 64:128]
                tt("dve", t1[:], x1, cb, ALU.mult, [qn, c], [t1])
                tt("pool", t2[:], x2, sb, ALU.mult, [qn, c], [t2])
                tt("dve", qb[:, :, 0:64], t1[:], t2[:], ALU.subtract, [t1, t2], [qb])
                tt("dve", t1[:], x1, sb, ALU.mult, [qn, c, qb], [t1])
                tt("pool", t2[:], x2, cb, ALU.mult, [qn, c, qb], [t2])
                tt("dve", qb[:, :, 64:128], t1[:], t2[:], ALU.add, [t1, t2], [qb])
            else:
                cp("dve", qb[:], qn[:], [qn], [qb])
            for q in range(2):
                bk = 6 + q
                nb = 8 if q == 0 else 2
                for jj in range(nb):
                    h = q * 8 + jj
                    tr(pbf(bk)[:, jj * 128:(jj + 1) * 128], qb[:, h, :], ident_bf[:], [qb, ident_bf], [PB[bk]])
                if q == 0:
                    cp("act", qT[:, :, rows(i)], pbf(bk).rearrange("p (a b) -> p a b", a=8), [PB[bk]], [qT])
                else:
                    cp("dve", kT[:, :, rows(i)], pbf(bk)[:, 0:256].rearrange("p (a b) -> p a b", a=2), [PB[bk]], [kT])
        A.release(m1)
        pbs = [A.alloc(f"pb{i}", [TOK], BF16) for i in range(2)]
        pT = A.alloc("pT", [NT, 128], BF16)
        sm = A.alloc("asm", [4], F32)
        omix = [A.alloc(f"omix{i}", [1024], BF16) for i in range(2)]
        scale = 128 ** -0.5
        n = 0
        for i in range(NT):
            nk = 256 if i < 2 else TOK
            nkt = nk // 128
            om = omix[i % 2]
            for h in range(8):
                g = h // 4
                pb = pbs[n % 2]; n += 1
                nch = (nk + 511) // 512
                for ch in range(nch):
                    cw = min(512, nk - ch * 512)
                    mm(pf(ch)[:, 0:cw], qT[:, h, rows(i)], kT[:, g, ch * 512:ch * 512 + cw], True, True, [qT, kT], [PB[ch]])
                sres = [PB[ch] for ch in range(nch)]
                gen("dve", "reduce_max", sres, [sm], out=sm[:, 0:1], in_=pf(0, 5)[:, 0:nk], axis=AX.X)
                ts("dve", sm[:, 1:2], sm[:, 0:1], -scale, None, ALU.mult, None, [sm], [sm])
                act(pb[:, 0:nk], pf(0, 5)[:, 0:nk], AF.Exp, sres + [sm], [pb, sm], bias=sm[:, 1:2], scale=scale, accum=sm[:, 2:3])
                recip(sm[:, 3:4], sm[:, 2:3], [sm], [sm])
                for q in range((nkt + 7) // 8):
                    bk = 6 + q % 2
                    nb = min(8, nkt - q * 8)
                    for jj in range(nb):
                        kt_ = q * 8 + jj
                        tr(pbf(bk)[:, jj * 128:(jj + 1) * 128], pb[:, kt_ * 128:(kt_ + 1) * 128], ident_bf[:], [pb, ident_bf], [PB[bk]])
                    cp("act" if q % 2 == 0 else "dve", pT[:, q * 8:q * 8 + nb, :], pbf(bk)[:, 0:nb * 128].rearrange("p (a b) -> p a b", a=nb), [PB[bk]], [pT])
                for kt_ in range(nkt):
                    mm(pf(5)[:, 0:128], pT[:, kt_, :], vA[:, kt_, g * 128:(g + 1) * 128], kt_ == 0, kt_ == nkt - 1, [pT, vA], [PB[5]])
                act(om[:, h * 128:(h + 1) * 128], pf(5)[:, 0:128], AF.Copy, [PB[5], sm], [om], scale=sm[:, 3:4])
            P.dma("pool", MIX[rows(i), 0:1024], om[:], reads=[om], writes=[MIX])
        A.release(m)

    def stage_s5(li):
        m = A.mark()
        uT = A.alloc("uT", [8, TOK], BF16)
        m1 = A.mark()
        ufs = [A.alloc(f"uf{i}", [1024], F32) for i in range(2)]
        ubs = [A.alloc(f"ub{i}", [1024], BF16) for i in range(2)]
        for i in range(NT):
            uf = ufs[i % 2]; ub = ubs[i % 2]
            P.dma("sync", uf[:], PROJ[rows(i), 1536:2560], reads=[PROJ], writes=[uf])
            cp("pool", ub[:], uf[:], [uf], [ub])
            for jj in range(8):
                tr(pbf(6 + i % 2)[:, jj * 128:(jj + 1) * 128], ub[:, jj * 128:(jj + 1) * 128], ident_bf[:], [ub, ident_bf], [PB[6 + i % 2]])
            cp("act", uT[:, :, rows(i)], pbf(6 + i % 2).rearrange("p (a b) -> p a b", a=8), [PB[6 + i % 2]], [uT])
        A.release(m1)
        par = [A.alloc(f"s5par{d}", [3, 32], F32) for d in range(2)]
        rr = [A.alloc(f"s5r{d}", [32], F32) for d in range(2)]
        th = [A.alloc(f"s5th{d}", [32], F32) for d in range(2)]
        cr = [A.alloc(f"s5cr{d}", [32], F32) for d in range(2)]
        ci = [A.alloc(f"s5ci{d}", [32], F32) for d in range(2)]
        pt = [A.alloc(f"s5pt{i}", [32], F32) for i in range(8)]
        pti = A.alloc("s5pti", [32], I32)

        def sincos(dst_sin, dst_cos, ang, wid, rres, tmpa, tmpk, tmpi):
            for (dst, shift) in ((dst_sin, 0.0), (dst_cos, math.pi / 2)):
                ts("dve", tmpa, ang, shift, 1.0 / TWO_PI, ALU.add, ALU.mult, rres, rres)
                cp("dve", tmpi, tmpa, rres, rres)
                cp("dve", tmpk, tmpi, rres, rres)
                stt("dve", tmpa, tmpk, -1.0, tmpa, ALU.mult, ALU.add, rres, rres)
                ts("dve", tmpa, tmpa, TWO_PI, math.pi, ALU.mult, ALU.min, rres, rres)
                ts("dve", tmpa, tmpa, -math.pi, None, ALU.max, None, rres, rres)
                act(dst, tmpa, AF.Sin, rres, rres)

        for d in range(2):
            R_ = [par[d], rr[d], th[d], cr[d], ci[d]] + pt + [pti]
            P.dma("sync", par[d][:].rearrange("p a b -> p (a b)"), s5_par[li, d], writes=[par[d]])
            are = par[d][:, 0, :]; aim = par[d][:, 1, :]
            dtv = pt[0][:]
            act(dtv, par[d][:, 2, :], AF.Exp, R_, R_)
            tt("dve", pt[1][:], are, dtv, ALU.mult, R_, R_)
            act(rr[d][:], pt[1][:], AF.Exp, R_, R_)
            tt("dve", th[d][:], aim, dtv, ALU.mult, R_, R_)
            sincos(pt[2][:], pt[3][:], th[d][:], 32, R_, pt[4][:], pt[5][:], pti[:])
            tt("dve", pt[4][:], rr[d][:], pt[3][:], ALU.mult, R_, R_)
            ts("dve", pt[4][:], pt[4][:], -1.0, None, ALU.add, None, R_, R_)
            tt("dve", pt[5][:], rr[d][:], pt[2][:], ALU.mult, R_, R_)
            tt("dve", pt[6][:], are, are, ALU.mult, R_, R_)
            tt("dve", pt[7][:], aim, aim, ALU.mult, R_, R_)
            tt("dve", pt[6][:], pt[6][:], pt[7][:], ALU.add, R_, R_)
            recip(pt[6][:], pt[6][:], R_, R_)
            tt("dve", pt[7][:], pt[4][:], are, ALU.mult, R_, R_)
            tt("dve", pt[1][:], pt[5][:], aim, ALU.mult, R_, R_)
            tt("dve", pt[7][:], pt[7][:], pt[1][:], ALU.add, R_, R_)
            tt("dve", cr[d][:], pt[7][:], pt[6][:], ALU.mult, R_, R_)
            tt("dve", pt[7][:], pt[5][:], are, ALU.mult, R_, R_)
            tt("dve", pt[1][:], pt[4][:], aim, ALU.mult, R_, R_)
            tt("dve", pt[7][:], pt[7][:], pt[1][:], ALU.subtract, R_, R_)
            tt("dve", ci[d][:], pt[7][:], pt[6][:], ALU.mult, R_, R_)
        spos = [A.alloc(f"spos{d}", [TOK], F32) for d in range(2)]
        for d in range(2):
            P.dma("sync", spos[d][:], k_spos[d], writes=[spos[d]])
        bw = [A.alloc(f"s5bw{i}", [2, 128], BF16) for i in range(2)]
        cw_ = [A.alloc(f"s5cw{i}", [2, 16], F32) for i in range(2)]
        cblk = [A.alloc(f"s5cb{i}", [2, 2, 32], BF16) for i in range(2)]
        ctmp = A.alloc("s5ct", [4, 16], F32)
        W = [A.alloc(f"s5w{i}", [TOK], F32) for i in range(8)]
        Wi = A.alloc("s5wi", [TOK], I32)
        hb = [A.alloc(f"s5h{i}", [TOK], BF16) for i in range(4)]
        ysb = A.alloc("s5y", [NT, 32], F32)
        bn = 0
        for j in range(32):
            cb = cblk[j % 2]
            gen("pool", "memset", [], [cb], ap=cb[:], constant=0.0)
            for d in range(2):
                b_ = bw[bn % 2]; c_ = cw_[bn % 2]; bn += 1
                P.dma("pool", b_[:, 0, :], s5_bblk[li, d, 0, j], writes=[b_])
                P.dma("pool", b_[:, 1, :], s5_bblk[li, d, 1, j], writes=[b_])
                P.dma("sync", c_[:, 0, :], s5_cblk[li, d, 0, :, j, :], writes=[c_])
                P.dma("sync", c_[:, 1, :], s5_cblk[li, d, 1, :, j, :], writes=[c_])
                crj = cr[d][:, j:j + 1]; cij = ci[d][:, j:j + 1]
                ts("dve", ctmp[:, 0, :], c_[:, 0, :], crj, None, ALU.mult, None, [c_, cr[d]], [ctmp])
                ts("dve", ctmp[:, 1, :], c_[:, 1, :], cij, None, ALU.mult, None, [c_, ci[d]], [ctmp])
                ts("dve", ctmp[:, 2, :], c_[:, 0, :], cij, None, ALU.mult, None, [c_, ci[d]], [ctmp])
                ts("dve", ctmp[:, 3, :], c_[:, 1, :], crj, None, ALU.mult, None, [c_, cr[d]], [ctmp])
                for q in range(2):
                    ps_ = slice(q * 64, (q + 1) * 64)
                    tt("dve", cb[ps_, d, 0, q * 16:(q + 1) * 16], ctmp[ps_, 0, :], ctmp[ps_, 1, :], ALU.subtract, [ctmp], [cb])
                    stt("dve", cb[ps_, d, 1, q * 16:(q + 1) * 16], ctmp[ps_, 2, :], -1.0, ctmp[ps_, 3, :], ALU.mult, ALU.subtract, [ctmp], [cb])
                ang, sn, cs_, xre, xim, wre, wim, tk = W
                RW = W + [Wi]
                ts("dve", ang[:], spos[d][:], th[d][:, j:j + 1], None, ALU.mult, None, [spos[d], th[d]] + RW, RW)
                sincos(sn[:], cs_[:], ang[:], TOK, RW, wre[:], tk[:], Wi[:])
                for part, dst in ((0, xre), (1, xim)):
                    for ch in range(5):
                        cw2 = min(512, TOK - ch * 512)
                        mm(pf(ch)[:, 0:cw2], b_[:, part, :], uT[:, j // 4, ch * 512:ch * 512 + cw2], True, True, [b_, uT], [PB[ch]])
                    cp("act", dst[:], pf(0, 5)[:, 0:TOK], [PB[c2] for c2 in range(5)], RW)
                tt("dve", wre[:], xre[:], cs_[:], ALU.mult, RW, RW)
                tt("pool", tk[:], xim[:], sn[:], ALU.mult, RW, RW)
                tt("dve", wre[:], wre[:], tk[:], ALU.add, RW, RW)
                tt("dve", wim[:], xim[:], cs_[:], ALU.mult, RW, RW)
                tt("pool", tk[:], xre[:], sn[:], ALU.mult, RW, RW)
                tt("dve", wim[:], wim[:], tk[:], ALU.subtract, RW, RW)
                rb = rr[d][:, j:j + 1].to_broadcast([128, TOK])
                for src, dst in ((wre, xre), (wim, xim)):
                    if d == 0:
                        gen("dve", "tensor_tensor_scan", RW + [rr[d]], RW, out=dst[:], data0=rb, data1=src[:], initial=0.0, op0=ALU.mult, op1=ALU.add)
                    else:
                        gen("dve", "tensor_tensor_scan", RW + [rr[d]], RW, out=dst[:, 0:256][:, ::-1], data0=rb[:, 0:256], data1=src[:, 0:256][:, ::-1], initial=0.0, op0=ALU.mult, op1=ALU.add)
                        gen("dve", "tensor_tensor_scan", RW + [rr[d]], RW, out=dst[:, 256:TOK][:, ::-1], data0=rb[:, 0:2048], data1=src[:, 256:TOK][:, ::-1], initial=dst[:, 0:1], op0=ALU.mult, op1=ALU.add)
                hre = hb[2 * d]; him = hb[2 * d + 1]
                tt("dve", wre[:], xre[:], cs_[:], ALU.mult, RW, RW)
                tt("pool", tk[:], xim[:], sn[:], ALU.mult, RW, RW)
                tt("dve", hre[:], wre[:], tk[:], ALU.subtract, RW + [hre], RW + [hre])
                tt("dve", wim[:], xre[:], sn[:], ALU.mult, RW, RW)
                tt("pool", tk[:], xim[:], cs_[:], ALU.mult, RW, RW)
                tt("dve", him[:], wim[:], tk[:], ALU.add, RW + [him], RW + [him])
            for i in range(NT):
                bk = 5
                col = (i % 16) * 32
                q = 0
                for d in range(2):
                    for part in range(2):
                        mm(pf(bk)[:, col:col + 32], hb[2 * d + part][:, rows(i)], cb[:, d, part, :], q == 0, q == 3, [hb[2 * d + part], cb], [PB[bk]])
                        q += 1
                if i % 16 == 15 or i == NT - 1:
                    lo = (i // 16) * 16
                    nn = i - lo + 1
                    cp("act", ysb[:, lo:lo + nn, :], pf(bk)[:, 0:nn * 32].rearrange("p (a b) -> p a b", a=nn), [PB[bk]], [ysb])
            P.dma("pool", YF.t[:, j * 32:(j + 1) * 32].rearrange("(n p) c -> p n c", p=128), ysb[:], reads=[ysb], writes=[YF])
        A.release(m)
        m = A.mark()
        db = A.alloc("s5db", [1024], F32)
        gbb = A.alloc("s5gbb", [1024], F32)
        P.dma("sync", db[:], bcast(s5_d[li:li + 1, :]), writes=[db])
        P.dma("sync", gbb[:], bcast(s5_glu_b[li:li + 1, :]), writes=[gbb])
        yfs = [A.alloc(f"ryf{i}", [1024], F32) for i in range(2)]
        ufs = [A.alloc(f"ruf{i}", [1024], F32) for i in range(2)]
        gbs = [A.alloc(f"rgb{i}", [1024], BF16) for i in range(2)]
        for i in range(NT):
            yf = yfs[i % 2]; uf = ufs[i % 2]; gb_ = gbs[i % 2]
            P.dma("sync", yf[:], YF[rows(i), :], reads=[YF], writes=[yf])
            P.dma("sync", uf[:], PROJ[rows(i), 1536:2560], reads=[PROJ], writes=[uf])
            tt("dve", uf[:], uf[:], db[:], ALU.mult, [uf, db], [uf])
            tt("pool", yf[:], yf[:], uf[:], ALU.add, [yf, uf], [yf])
            act(gb_[:], yf[:], AF.Gelu, [yf], [gb_])
            P.dma("pool", GT5[rows(i), :], gb_[:], reads=[gb_], writes=[GT5])
        A.release(m)
        stage_transpose(GT5, GT5T, 1024)

        def epi_glu():
            gts = [A.alloc(f"gg{i}", [1024], BF16) for i in range(2)]
            sg = [A.alloc(f"gs{i}", [1024], F32) for i in range(2)]
            ob = [A.alloc(f"go{i}", [1024], BF16) for i in range(2)]
            gbb2 = A.alloc("gbb2", [1024], F32)
            P.dma("sync", gbb2[:], bcast(s5_glu_b[li:li + 1, :]), writes=[gbb2])
            st = {"n": 0}

            def epi(i, c0, gw, psap, pres):
                k = st["n"] % 2; st["n"] += 1
                P.dma("sync", gts[k][:], GT5[rows(i), :], reads=[GT5], writes=[gts[k]])
                tt("dve", sg[k][:, 0:gw], psap, gbb2[:, c0:c0 + gw], ALU.add, pres + [gbb2], [sg[k]])
                act(sg[k][:, 0:gw], sg[k][:, 0:gw], AF.Sigmoid, [sg[k]], [sg[k]])
                tt("dve", ob[k][:, 0:gw], sg[k][:, 0:gw], gts[k][:, c0:c0 + gw], ALU.mult, [sg[k], gts[k]], [ob[k]])
                P.dma("pool", MIX[rows(i), 1024 + c0:1024 + c0 + gw], ob[k][:, 0:gw], reads=[ob[k]], writes=[MIX])
            return epi
        stage_linear(GT5T, 8, s5_glu_w[li], 1024, epi_glu)

    def stage_attn_c(li):
        m = A.mark()
        qT = A.alloc("cqT", [16, TOK], BF16, parts=64)
        kT = A.alloc("ckT", [2, TOK], BF16, parts=64)
        vC = A.alloc("cvC", [NT, 128], BF16)
        sinkb = A.alloc("sinkb", [16], F32)
        maskw = A.alloc("maskw", [384], F32)
        P.dma("sync", sinkb[:], bcast(c_sink[li:li + 1, :]), writes=[sinkb])
        P.dma("sync", maskw[:], k_maskw, writes=[maskw])
        m1 = A.mark()
        qks = [A.alloc(f"cqk{i}", [18, 64], F32) for i in range(2)]
        qb = A.alloc("cqb", [18, 64], BF16)
        cs = [A.alloc(f"ccs{i}", [2, 32], F32) for i in range(2)]
        t1 = A.alloc("crt1", [18, 32], F32); t2 = A.alloc("crt2", [18, 32], F32)
        vf = [A.alloc(f"cvf{i}", [128], F32) for i in range(2)]
        for i in range(NT):
            qk = qks[i % 2]
            P.dma("sync", qk[:].rearrange("p a b -> p (a b)"), PROJ[rows(i), 0:1152], reads=[PROJ], writes=[qk])
            P.dma("sync", vf[i % 2][:], PROJ[rows(i), 1152:1280], reads=[PROJ], writes=[vf[i % 2]])
            cp("pool", vC[:, i, :], vf[i % 2][:], [vf[i % 2]], [vC])
            if i >= 2:
                c = cs[i % 2]
                P.dma("sync", c[:, 0, :], k_rope[2, rows(i - 2), 0:32], writes=[c])
                P.dma("sync", c[:, 1, :], k_rope[3, rows(i - 2), 0:32], writes=[c])
                cb = c[:, 0, :].unsqueeze(1).to_broadcast([128, 18, 32])
                sb = c[:, 1, :].unsqueeze(1).to_broadcast([128, 18, 32])
                x1 = qk[:, :, 0:32]; x2 = qk[:, :, 32:64]
                tt("dve", t1[:], x1, cb, ALU.mult, [qk, c], [t1])
                tt("pool", t2[:], x2, sb, ALU.mult, [qk, c], [t2])
                tt("dve", qb[:, :, 0:32], t1[:], t2[:], ALU.subtract, [t1, t2], [qb])
                tt("dve", t1[:], x1, sb, ALU.mult, [qk, c, qb], [t1])
                tt("pool", t2[:], x2, cb, ALU.mult, [qk, c, qb], [t2])
                tt("dve", qb[:, :, 32:64], t1[:], t2[:], ALU.add, [t1, t2], [qb])
            else:
                cp("dve", qb[:], qk[:], [qk], [qb])
            for q in range(3):
                bk = 5 + q
                nb = 8 if q < 2 else 2
                for jj in range(nb):
                    h = q * 8 + jj
                    tr(pbf(bk)[0:64, jj * 128:(jj + 1) * 128], qb[:, h, :], ident_bf[:], [qb, ident_bf], [PB[bk]])
                if q < 2:
                    cp("act" if q == 0 else "dve", qT[:, q * 8:(q + 1) * 8, rows(i)], pbf(bk)[0:64, :].rearrange("p (a b) -> p a b", a=8), [PB[bk]], [qT])
                else:
                    cp("dve", kT[:, :, rows(i)], pbf(bk)[0:64, 0:256].rearrange("p (a b) -> p a b", a=2), [PB[bk]], [kT])
        A.release(m1)
        Sm = A.alloc("cSm", [640], F32)
        pbs = [A.alloc(f"cpb{i}", [640], BF16) for i in range(2)]
        pT = A.alloc("cpT", [5, 128], BF16)
        sm = A.alloc("csm", [8], F32)
        omix = [A.alloc(f"comix{i}", [1024], BF16) for i in range(2)]
        n = 0
        for i in range(NT):
            om = omix[i % 2]
            if i >= 2:
                l_ = i - 2
                lo = max(l_ - 1, 0); hi = min(l_ + 1, 15)
                w = (hi - lo + 1) * 128
                k0 = 256 + lo * 128
                mcol = 128 if l_ == 0 else 0
            else:
                w = 0
            nk = 256 + w
            nkt = nk // 128
            for h in range(16):
                g = h // 8
                pb = pbs[n % 2]; n += 1
                mm(pf(0)[:, 0:256], qT[:, h, rows(i)], kT[:, g, 0:256], True, True, [qT, kT], [PB[0]])
                act(Sm[:, 0:256], pf(0)[:, 0:256], AF.Copy, [PB[0]], [Sm], scale=0.125)
                if w:
                    mm(pf(1)[:, 0:w], qT[:, h, rows(i)], kT[:, g, k0:k0 + w], True, True, [qT, kT], [PB[1]])
                    stt("dve", Sm[:, 256:256 + w], pf(1)[:, 0:w], 0.125, maskw[:, mcol:mcol + w], ALU.mult, ALU.add, [PB[1], maskw], [Sm])
                gen("dve", "reduce_max", [Sm], [sm], out=sm[:, 0:1], in_=Sm[:, 0:nk], axis=AX.X)
                tt("dve", sm[:, 0:1], sm[:, 0:1], sinkb[:, h:h + 1], ALU.max, [sm, sinkb], [sm])
                ts("dve", sm[:, 1:2], sm[:, 0:1], -1.0, None, ALU.mult, None, [sm], [sm])
                act(pb[:, 0:nk], Sm[:, 0:nk], AF.Exp, [Sm, sm], [pb, sm], bias=sm[:, 1:2], accum=sm[:, 2:3])
                act(sm[:, 4:5], sinkb[:, h:h + 1], AF.Exp, [sinkb, sm], [sm], bias=sm[:, 1:2])
                tt("dve", sm[:, 2:3], sm[:, 2:3], sm[:, 4:5], ALU.add, [sm], [sm])
                recip(sm[:, 3:4], sm[:, 2:3], [sm], [sm])
                bk = 6 + h % 2
                for kt_ in range(nkt):
                    tr(pbf(bk)[:, kt_ * 128:(kt_ + 1) * 128], pb[:, kt_ * 128:(kt_ + 1) * 128], ident_bf[:], [pb, ident_bf], [PB[bk]])
                cp("act" if h % 2 == 0 else "dve", pT[:, 0:nkt, :], pbf(bk)[:, 0:nkt * 128].rearrange("p (a b) -> p a b", a=nkt), [PB[bk]], [pT])
                for kt_ in range(nkt):
                    tile_ = kt_ if kt_ < 2 else 2 + lo + (kt_ - 2)
                    mm(pf(5)[:, 0:64], pT[:, kt_, :], vC[:, tile_, g * 64:(g + 1) * 64], kt_ == 0, kt_ == nkt - 1, [pT, vC], [PB[5]])
                act(om[:, h * 64:(h + 1) * 64], pf(5)[:, 0:64], AF.Copy, [PB[5], sm], [om], scale=sm[:, 3:4])
            P.dma("pool", MIX[rows(i), 0:1024], om[:], reads=[om], writes=[MIX])
        A.release(m)

    def stage_mamba(li, upto=99):
        m = A.mark()
        wb = [A.alloc(f"mcw{i}", [1536], F32) for i in range(4)]
        for k in range(3):
            P.dma("sync", wb[k][:], bcast(m_conv_w[li, k:k + 1, :]), writes=[wb[k]])
        P.dma("sync", wb[3][:], bcast(m_conv_b[li:li + 1, :]), writes=[wb[3]])
        dtb = A.alloc("mdtb", [32], F32); ab = A.alloc("mab", [32], F32)
        P.dma("sync", dtb[:], bcast(m_dt_bias[li:li + 1, :]), writes=[dtb])
        P.dma("sync", ab[:], bcast(m_a_log[li:li + 1, :]), writes=[ab])
        act(ab[:], ab[:], AF.Exp, [ab], [ab])
        ts("dve", ab[:], ab[:], -1.0, None, ALU.mult, None, [ab], [ab])
        xms = [A.alloc(f"mxm{i}", [1536], F32) for i in range(2)]
        xps = [A.alloc(f"mxp{i}", [1536], F32) for i in range(2)]
        xns = [A.alloc(f"mxn{i}", [1536], F32) for i in range(2)]
        acc = A.alloc("macc", [1536], F32); tq = A.alloc("mtq", [1536], F32)
        xcb = [A.alloc(f"mxcb{i}", [1536], BF16) for i in range(2)]
        dts = [A.alloc(f"mdt{i}", [64], F32) for i in range(2)]
        for i in range(NT):
            r0 = i * 128
            seg_lo = 0 if i < 2 else 256
            seg_hi = 256 if i < 2 else TOK
            xm = xms[i % 2]; xp = xps[i % 2]; xn = xns[i % 2]; xc_ = xcb[i % 2]; dt_ = dts[i % 2]
            P.dma("sync", xm[:], PROJ[r0:r0 + 128, 2304:3840], reads=[PROJ], writes=[xm])
            if r0 == seg_lo:
                gen("pool", "memset", [], [xp], ap=xp[:], constant=0.0)
                P.dma("sync", xp[1:128, :], PROJ[r0:r0 + 127, 2304:3840], reads=[PROJ], writes=[xp])
            else:
                P.dma("sync", xp[:], PROJ[r0 - 1:r0 + 127, 2304:3840], reads=[PROJ], writes=[xp])
            if r0 + 128 == seg_hi:
                gen("pool", "memset", [], [xn], ap=xn[:], constant=0.0)
                P.dma("sync", xn[0:127, :], PROJ[r0 + 1:r0 + 128, 2304:3840], reads=[PROJ], writes=[xn])
            else:
                P.dma("sync", xn[:], PROJ[r0 + 1:r0 + 129, 2304:3840], reads=[PROJ], writes=[xn])
            tt("dve", acc[:], xm[:], wb[1][:], ALU.mult, [xm, wb[1]], [acc])
            tt("pool", tq[:], xp[:], wb[0][:], ALU.mult, [xp, wb[0]], [tq])
            tt("dve", acc[:], acc[:], tq[:], ALU.add, [acc, tq], [acc])
            tt("pool", tq[:], xn[:], wb[2][:], ALU.mult, [xn, wb[2], acc], [tq])
            tt("dve", acc[:], acc[:], tq[:], ALU.add, [acc, tq], [acc])
            tt("dve", acc[:], acc[:], wb[3][:], ALU.add, [acc, wb[3]], [acc])
            act(xc_[:], acc[:], AF.Silu, [acc], [xc_])
            P.dma("pool", XC[rows(i), :], xc_[:], reads=[xc_], writes=[XC])
            P.dma("sync", dt_[:, 0:32], PROJ[rows(i), 3840:3872], reads=[PROJ], writes=[dt_])
            tt("dve", dt_[:, 0:32], dt_[:, 0:32], dtb[:], ALU.add, [dt_, dtb], [dt_])
            act(dt_[:, 0:32], dt_[:, 0:32], AF.Exp, [dt_], [dt_])
            act(dt_[:, 0:32], dt_[:, 0:32], AF.Ln, [dt_], [dt_], bias=1.0)
            tt("dve", dt_[:, 32:64], dt_[:, 0:32], ab[:], ALU.mult, [dt_, ab], [dt_])
            P.dma("pool", DTD[rows(i), :], dt_[:], reads=[dt_], writes=[DTD])
            import os as _os2
            if not _os2.environ.get("NO_SZ"):
                P.dma("sync", acc[:, 0:1024], PROJ[rows(i), 1280:2304], reads=[PROJ, xc_], writes=[acc])
                act(tq[:, 0:1024], acc[:, 0:1024], AF.Silu, [acc], [tq])
                P.dma("pool", SZ[rows(i), :], tq[:, 0:1024], reads=[tq], writes=[SZ])
        A.release(m)
        if upto < 2:
            return
        m = A.mark()
        H = A.alloc("mH", [2, 512], F32); Hb = A.alloc("mHb", [2, 512], BF16)
        TRI = [A.alloc(f"mtri{d}", [128], F32) for d in range(2)]
        NEGM = [A.alloc(f"mneg{d}", [128], F32) for d in range(2)]
        onesf = A.alloc("monesf", [128], F32)
        for d in range(2):
            P.dma("sync", TRI[d][:], k_tri[d], writes=[TRI[d]])
            P.dma("sync", NEGM[d][:], k_tri[2 + d], writes=[NEGM[d]])
        gen("dve", "memset", [], [onesf], ap=onesf[:], constant=1.0)
        warm = A.alloc("mwarm", [8], F32)
        gen("dve", "memset", [], [warm], ap=warm[:], constant=0.0)
        act(warm[:], warm[:], AF.Exp, [warm], [warm])
        Db = A.alloc("mDb", [16], F32); ngb = A.alloc("mngb", [1024], F32)
        P.dma("sync", Db[:], bcast(m_d[li:li + 1, :]), writes=[Db])
        P.dma("sync", ngb[:], bcast(m_norm_g[li:li + 1, :]), writes=[ngb])
        xss = [A.alloc(f"mxs{i}", [24, 64], BF16) for i in range(2)]
        dtd = [A.alloc(f"mdtd{i}", [64], F32) for i in range(2)]
        rhsM = A.alloc("mrhsM", [16, 128], F32)
        acs = A.alloc("macs", [16], F32); eacs = A.alloc("meacs", [16], F32); cdb = A.alloc("mcdb", [16], F32)
        seg = A.alloc("mseg", [16, 128], F32)
        BCT = A.alloc("mBCT", [4, 128], BF16)
        Wm = A.alloc("mW", [16, 128], BF16)
        xdt = A.alloc("mxdt", [16, 64], BF16); xdd = A.alloc("mxdd", [16, 64], BF16)
        yb = [A.alloc(f"my{i}", [16, 64], F32) for i in range(2)]
        yfl = [A.alloc(f"myf{i}", [1024], F32) for i in range(2)]
        zf = [A.alloc(f"mzf{i}", [1024], F32) for i in range(2)]
        t2 = A.alloc("mt2", [16, 64], F32)
        junk = A.alloc("mjunk", [1024], F32)
        ss = A.alloc("mss", [2], F32)
        ob = [A.alloc(f"mob{i}", [1024], BF16) for i in range(2)]
        n = 0
        import os as _os
        for d in [int(ch) for ch in _os.environ.get('MAMBA_DIRS', '01')]:
            order = list(range(NT)) if d == 0 else [1, 0] + list(range(NT - 1, 1, -1))
            LAST = 127 if d == 0 else 0
            gen("dve", "memset", [], [H], ap=H[:], constant=0.0)
            gen("dve", "memset", [], [Hb], ap=Hb[:], constant=0.0)
            for c in order:
                xs_ = xss[n % 2]; dd = dtd[n % 2]; y_ = yb[n % 2]; yf_ = yfl[n % 2]; z_ = zf[n % 2]; o_ = ob[n % 2]
                n += 1
                P.dma("sync", xs_[:].rearrange("p a b -> p (a b)"), XC[rows(c), :], reads=[XC], writes=[xs_])
                P.dma("sync", dd[:], DTD[rows(c), :], reads=[DTD], writes=[dd])
                xs3 = xs_[:, 0:16, :]
                Bb = xs_[:, 16:20, :].rearrange("p (g a) b -> p g (a b)", g=2)
                Cb = xs_[:, 20:24, :].rearrange("p (g a) b -> p g (a b)", g=2)
                dt_ = dd[:, d * 16:(d + 1) * 16]
                dA = dd[:, 32 + d * 16:32 + (d + 1) * 16]
                tt("dve", rhsM[:], TRI[d][:].unsqueeze(1).to_broadcast([128, 16, 128]), dA.unsqueeze(2).to_broadcast([128, 16, 128]), ALU.mult, [TRI[d], dd], [rhsM])
                for q in range(4):
                    mm(pf(q), onesf[:], rhsM[:, 4 * q:4 * q + 4, :].rearrange("p a b -> p (a b)"), True, True, [onesf, rhsM], [PB[q]])
                mm(pf(4)[:, 0:16], TRI[d][:], dA, True, True, [TRI[d], dd], [PB[4]])
                cp("act", acs[:], pf(4)[:, 0:16], [PB[4]], [acs])
                acsb = pf(0, 4).rearrange("p (a b) -> p a b", a=16)
                tt("dve", seg[:], acsb, acs[:].unsqueeze(2).to_broadcast([128, 16, 128]), ALU.subtract, [PB[0], PB[1], PB[2], PB[3], acs], [seg])
                act(cdb[:], acsb[:, :, LAST], AF.Exp, [PB[0], PB[1], PB[2], PB[3]], [cdb])
                tt("pool", seg[:], seg[:], NEGM[d][:].unsqueeze(1).to_broadcast([128, 16, 128]), ALU.add, [seg, NEGM[d]], [seg])
                act(seg[:], seg[:], AF.Exp, [seg], [seg])
                act(eacs[:], acs[:], AF.Exp, [acs], [eacs])
                if upto < 3:
                    continue
                for q in range(2):
                    for g in range(2):
                        src = Bb if q == 0 else Cb
                        tr(pbf(6)[:, (q * 2 + g) * 128:(q * 2 + g + 1) * 128], src[:, g, :], ident_bf[:], [xs_, ident_bf], [PB[6]])
                cp("dve", BCT[:], pbf(6)[:, 0:512].rearrange("p (a b) -> p a b", a=4), [PB[6]], [BCT])
                for g in range(2):
                    mm(pf(5)[:, g * 128:(g + 1) * 128], BCT[:, g, :], BCT[:, 2 + g, :], True, True, [BCT], [PB[5]])
                for g in range(2):
                    tt("dve", Wm[:, g * 8:(g + 1) * 8, :], seg[:, g * 8:(g + 1) * 8, :], pf(5)[:, g * 128:(g + 1) * 128].unsqueeze(1).to_broadcast([128, 8, 128]), ALU.mult, [seg, PB[5]], [Wm])
                if upto < 4:
                    continue
                tt("pool", xdt[:], xs3, dt_.unsqueeze(2).to_broadcast([128, 16, 64]), ALU.mult, [xs_, dd], [xdt])
                tt("pool", xdd[:], xdt[:], seg[:, :, LAST].unsqueeze(2).to_broadcast([128, 16, 64]), ALU.mult, [xdt, seg], [xdd])
                for h in range(16):
                    bk = h // 8
                    mm(pf(bk)[:, (h % 8) * 64:(h % 8 + 1) * 64], Wm[:, h, :], xdt[:, h, :], True, True, [Wm, xdt], [PB[bk]])
                for g in range(2):
                    mm(pf(2 + g), BCT[:, 2 + g, :], Hb[:, g, :], True, True, [BCT, Hb], [PB[2 + g]])
                tt("dve", y_[:], pf(2, 2).rearrange("p (a b) -> p a b", a=16), eacs[:].unsqueeze(2).to_broadcast([128, 16, 64]), ALU.mult, [PB[2], PB[3], eacs], [y_])
                tt("dve", y_[:], y_[:], pf(0, 2).rearrange("p (a b) -> p a b", a=16), ALU.add, [y_, PB[0], PB[1]], [y_])
                if upto < 5:
                    continue
                sbk = [4, 7]
                for g in range(2):
                    mm(pf(sbk[g]), Bb[:, g, :], xdd[:, g * 8:(g + 1) * 8, :].rearrange("p a b -> p (a b)"), True, True, [xs_, xdd], [PB[sbk[g]]])
                H4 = H[:].rearrange("p g (a b) -> p g a b", a=8)
                for g in range(2):
                    tt("dve", H4[:, g], H4[:, g], cdb[:, g * 8:(g + 1) * 8].unsqueeze(2).to_broadcast([128, 8, 64]), ALU.mult, [H, cdb, Hb], [H])
                    tt("dve", H[:, g, :], H[:, g, :], pf(sbk[g]), ALU.add, [H, PB[sbk[g]]], [H])
                cp("pool", Hb[:], H[:], [H], [Hb])
                if upto < 6:
                    continue
                P.dma(_os.environ.get("MAMBA_STQ", "sync"), (YF if d == 0 else YB)[rows(c), :], y_[:].rearrange("p a b -> p (a b)"), reads=[y_], writes=[YF if d == 0 else YB])
        A.release(m)
        if upto < 7:
            return
        m = A.mark()
        Db = A.alloc("rDb", [16], F32); ngb = A.alloc("rngb", [1024], F32)
        P.dma("sync", Db[:], bcast(m_d[li:li + 1, :]), writes=[Db])
        P.dma("sync", ngb[:], bcast(m_norm_g[li:li + 1, :]), writes=[ngb])
        yfs = [A.alloc(f"ryf{i}", [1024], F32) for i in range(2)]
        ybs = [A.alloc(f"ryb{i}", [1024], F32) for i in range(2)]
        zs = [A.alloc(f"rz{i}", [1024], F32) for i in range(2)]
        xsb = [A.alloc(f"rxs{i}", [1024], BF16) for i in range(2)]
        t2 = A.alloc("rt2", [16, 64], F32)
        junk = A.alloc("rjunk", [1024], F32)
        sss = [A.alloc(f"rss{i}", [2], F32) for i in range(2)]
        obs = [A.alloc(f"rob{i}", [1024], BF16) for i in range(2)]
        for i in range(NT):
            yf_ = yfs[i % 2]; yb_ = ybs[i % 2]; z_ = zs[i % 2]; xs_ = xsb[i % 2]; ss = sss[i % 2]; o_ = obs[i % 2]
            P.dma("sync", yf_[:], YF[rows(i), :], reads=[YF], writes=[yf_])
            P.dma("sync", yb_[:], YB[rows(i), :], reads=[YB], writes=[yb_])
            P.dma("sync", z_[:], SZ[rows(i), :], reads=[SZ], writes=[z_])
            P.dma("sync", xs_[:], XC[rows(i), 0:1024], reads=[XC], writes=[xs_])
            tt("dve", yf_[:], yf_[:], yb_[:], ALU.add, [yf_, yb_], [yf_])
            tt("pool", t2[:], xs_[:].rearrange("p (a b) -> p a b", a=16), Db[:].unsqueeze(2).to_broadcast([128, 16, 64]), ALU.mult, [xs_, Db], [t2])
            tt("dve", yf_[:], yf_[:], t2[:].rearrange("p a b -> p (a b)"), ALU.add, [yf_, t2], [yf_])
            tt("dve", yf_[:], yf_[:], z_[:], ALU.mult, [yf_, z_], [yf_])
            act(junk[:], yf_[:], AF.Square, [yf_], [junk, ss], accum=ss[:, 0:1])
            ts("dve", ss[:, 1:2], ss[:, 0:1], 1.0 / 1024, EPS, ALU.mult, ALU.add, [ss], [ss])
            act(ss[:, 1:2], ss[:, 1:2], AF.Sqrt, [ss], [ss])
            recip(ss[:, 1:2], ss[:, 1:2], [ss], [ss])
            stt("dve", o_[:], yf_[:], ss[:, 1:2], ngb[:], ALU.mult, ALU.mult, [yf_, ss, ngb], [o_])
            P.dma("sync", MIX[rows(i), 1024:2048], o_[:], reads=[o_], writes=[MIX])
        A.release(m)

    dbg_outs = {}

    def dump(name, src, shape, dtype):
        o = nc.dram_tensor("dbg_" + name, list(shape), dtype, kind="ExternalOutput").ap()
        P.barrier()
        P.dma("sync", o, src, reads=[])
        P.barrier()

    if unit == "mamba":
        proj_in = inp("proj_in", [TOK, 3872])
        for i in range(NT):
            P.dma("sync", PROJ[rows(i), :], proj_in[rows(i), :], writes=[PROJ])
        P.barrier()
        stage_mamba(0, upto)
        P.barrier()
        if upto >= 9:
            dump("mix", MIX.t[:, 1024:2048], [TOK, 1024], BF16)
        dump("xc", XC.t, [TOK, 1536], BF16)
        dump("dtd", DTD.t, [TOK, 64], F32)
        if upto >= 6:
            dump("yf", YF.t, [TOK, 1024], F32)
        P.emit()
        return nc
    for l in range(n_layers):
        li = l // 2
        stage_mod(l)
        stage_norm(norm_g[2 * l:2 * l + 1, :], 0, 1, HT, None)
        P.barrier()
        if l % 2 == 0:
            stage_linear(HT, 16, ev_w_in[li], 2560, epi_store(PROJ))
            P.barrier()
            if dbg and l == dbg.get("layer", -1) and "proj" in dbg["what"]:
                dump("proj", PROJ.t, [TOK, 3872], F32)
            stage_attn_a(li)
            stage_s5(li)
            w_out = ev_w_out[li]
        else:
            stage_linear(HT, 16, od_w_in[li], 3872, epi_store(PROJ))
            P.barrier()
            import os
            if not os.environ.get('SKIP_C'):
                stage_attn_c(li)
            if not os.environ.get('SKIP_M'):
                stage_mamba(li)
            w_out = od_w_out[li]
        P.barrier()
        if dbg and l == dbg.get("layer", -1) and "mix" in dbg["what"]:
            dump("mix", MIX.t, [TOK, D], BF16)
        stage_transpose(MIX, MIXT, 2048)
        stage_linear(MIXT, 16, w_out, 2048, epi_resid(2))
        P.barrier()
        if dbg and l == dbg.get("layer", -1) and "xmid" in dbg["what"]:
            dump("xmid", X.t, [TOK, D], F32)
        stage_norm(norm_g[2 * l + 1:2 * l + 2, :], 3, 4, HT, HTOK)
        P.barrier()
        stage_moe(l)
        P.barrier()
        if dbg and l == dbg.get("layer", -1) and "xout" in dbg["what"]:
            dump("xout", X.t, [TOK, D], F32)
    stage_norm(final_norm_g[0:1, :], 0, 0, None, None, tiles=range(2, NT), final_out=y_out)
    P.emit()
    return nc


def host_consts():
    c = {}
    c["k_ident"] = np.eye(128, dtype=np.float32)
    c["k_iota"] = np.tile(np.arange(256, dtype=np.float32)[None, :], (128, 1))
    p = np.arange(128, dtype=np.float32)
    c["k_iotap"] = np.stack([p, p + 128], axis=1).astype(np.float32)
    t = np.arange(TOK, dtype=np.float32)
    sb = np.where(t < 256, 255 - t, 256 + (2303 - t)).astype(np.float32)
    c["k_spos"] = np.stack([np.tile(t[None], (128, 1)), np.tile(sb[None], (128, 1))]).astype(np.float32)
    rope = np.zeros((4, 2048, 64), np.float32)
    tt_ = np.arange(2048)
    row = (tt_ // 64).astype(np.float32); col = (tt_ % 64).astype(np.float32)
    for idx, hd in ((0, 128), (2, 64)):
        nf = hd // 4
        inv = (10000.0 ** (-np.arange(nf, dtype=np.float32) / nf)).astype(np.float32)
        ang = np.concatenate([row[:, None] * inv, col[:, None] * inv], axis=-1).astype(np.float32)
        rope[idx, :, :hd // 2] = np.cos(ang)
        rope[idx + 1, :, :hd // 2] = np.sin(ang)
    c["k_rope"] = rope
    r = np.arange(128)[:, None]; j = np.arange(128)[None, :]
    mw = np.zeros((128, 384), np.float32)
    mw[:, 0:128] = np.where(j >= r, 0.0, -1e30)
    mw[:, 256:384] = np.where(j <= r, 0.0, -1e30)
    c["k_maskw"] = mw
    s = np.arange(128)[:, None]; i = np.arange(128)[None, :]
    tri = np.zeros((4, 128, 128), np.float32)
    tri[0] = (s <= i); tri[1] = (s >= i)
    tri[2] = np.where(s <= i, 0.0, -1e30); tri[3] = np.where(s >= i, 0.0, -1e30)
    c["k_tri"] = tri
    return c


def kernel(**inputs):
    inp = {k: np.asarray(v) for k, v in inputs.items()}
    nc = build_program()
    shared = host_consts()
    f = lambda a: np.ascontiguousarray(a, dtype=np.float32)
    for k in ["mod_w", "mod_b", "ev_w_in", "ev_w_out", "a_q_norm", "a_k_norm", "s5_d", "s5_glu_w", "s5_glu_b", "od_w_in", "od_w_out",
              "c_sink", "m_conv_w", "m_conv_b", "m_d", "m_norm_g", "moe_router", "moe_w_gate", "moe_w_up", "moe_w_down"]:
        shared[k] = f(inp[k])
    shared["norm_g"] = f(inp["norm_g"].reshape(8, D))
    shared["m_dt_bias"] = f(inp["m_dt_bias"].reshape(2, 32))
    shared["m_a_log"] = f(inp["m_a_log"].reshape(2, 32))
    shared["final_norm_g"] = f(inp["final_norm_g"].reshape(1, D))
    are = inp["s5_a_re"].reshape(2, 2, 32, 2, 64)
    aim = inp["s5_a_im"].reshape(2, 2, 32, 2, 64)
    ldt = np.broadcast_to(inp["s5_log_dt"].reshape(2, 2, 32, 2, 1), (2, 2, 32, 2, 64))
    par = np.stack([are, aim, ldt], axis=2)
    shared["s5_par"] = f(par.transpose(0, 1, 4, 5, 2, 3).reshape(2, 2, 128, 96))
    bblk = np.zeros((2, 2, 2, 32, 128, 128), np.float32)
    for part, key in ((0, "s5_b_re"), (1, "s5_b_im")):
        b = inp[key].reshape(2, 2, 32, 2, 64, 16)
        for jj in range(32):
            for q in range(2):
                u0 = 32 * (jj % 4) + q * 16
                bblk[:, :, part, jj, u0:u0 + 16, q * 64:(q + 1) * 64] = b[:, :, jj, q].transpose(0, 1, 3, 2)
    shared["s5_bblk"] = bblk
    cb = np.stack([inp["s5_c_re"], inp["s5_c_im"]], axis=2).reshape(2, 2, 2, 32, 2, 16, 64)
    shared["s5_cblk"] = f(cb.transpose(0, 1, 2, 4, 6, 3, 5).reshape(2, 2, 2, 128, 32, 16))
    in_maps = []
    for b in range(8):
        mp = dict(shared)
        mp["x_in"] = f(inp["x"][b]); mp["ctx_in"] = f(inp["ctx"][b])
        cT = np.stack([inp["c"][b].reshape(16, 128).T, inp["c_ctx"].reshape(16, 128).T], axis=-1)
        mp["cT_in"] = f(cT.reshape(128, 32))
        in_maps.append(mp)
    res = run_bass_kernel_spmd(nc, in_maps, core_ids=list(range(8)))
    return np.stack([r["y_out"] for r in res.results], axis=0).astype(np.float32)
```

```python
import math
import numpy as np
import concourse.bass as bass
import concourse.mybir as mybir
from concourse.bass_utils import run_bass_kernel_spmd
from contextlib import ExitStack

F32 = mybir.dt.float32
BF16 = mybir.dt.bfloat16
I32 = mybir.dt.int32
U32 = mybir.dt.uint32
ALU = mybir.AluOpType
AF = mybir.ActivationFunctionType
AX = mybir.AxisListType

EPOCH = 24000
NSLOT = 8


class Res:
    __slots__ = ("writers", "readers", "multi", "name", "phase")

    def __init__(self, name="", multi=False):
        self.writers = []
        self.readers = []
        self.multi = multi
        self.name = name
        self.phase = []


class T:
    def __init__(self, t, name, multi=False):
        self.t = t
        self.res = Res(name, multi)
        self.name = name
        self._sub = {}

    def __getitem__(self, k):
        return self.t[k]

    def sub(self, key):
        if key not in self._sub:
            self._sub[key] = Res(f"{self.name}/{key}", False)
        return self._sub[key]


def _res(x):
    return x.res if isinstance(x, T) else x


class Prog:
    ENGS = ("sync", "act", "dve", "pool", "pe")

    def __init__(self, nc):
        self.nc = nc
        self.ops = []
        self.n_comp = {e: 0 for e in self.ENGS}
        self.n_dma = {e: 0 for e in self.ENGS}
        self.stack = ExitStack()
        self.eng_ops = {e: [] for e in self.ENGS}

    def sb(self, name, shape, dtype, multi=False):
        t = self.stack.enter_context(self.nc.sbuf_tensor(name, list(shape), dtype))
        return T(t, name, multi)

    def ps(self, name, shape, dtype):
        t = self.stack.enter_context(self.nc.psum_tensor(name, list(shape), dtype))
        return T(t, name)

    def dram(self, name, shape, dtype, kind="Internal", multi=True):
        t = self.nc.dram_tensor(name, list(shape), dtype, kind=kind)
        return T(t.ap(), name, multi)

    def _record(self, eng, fn, reads, writes, is_dma):
        deps = []
        rs = [_res(r) for r in reads]
        ws = [_res(w) for w in writes]
        for r in rs:
            deps.extend(r.writers)
        op_id = len(self.ops)
        for w in ws:
            if w.multi:
                if w.readers:
                    w.phase = w.readers + w.writers
                    w.writers = []
                    w.readers = []
                deps.extend(w.phase)
            else:
                deps.extend(w.readers)
                deps.extend(w.writers)
        if is_dma:
            k = self.n_dma[eng]
            self.n_dma[eng] += 1
            tok = ("d", eng, k)
        else:
            k = self.n_comp[eng]
            self.n_comp[eng] += 1
            tok = ("c", eng, k)
        op = dict(eng=eng, fn=fn, deps=set(deps), tok=tok, is_dma=is_dma)
        self.ops.append(op)
        self.eng_ops[eng].append(op_id)
        for r in rs:
            r.readers.append(op_id)
        for w in ws:
            if w.multi:
                w.writers.append(op_id)
            else:
                w.writers = [op_id]
                w.readers = []
        return op_id

    def barrier(self):
        snap = (dict(self.n_comp), dict(self.n_dma))
        for e in self.ENGS:
            op = dict(eng=e, fn=None, deps=set(), tok=None, is_dma=False, barrier=snap)
            self.ops.append(op)
            self.eng_ops[e].append(len(self.ops) - 1)

    def op(self, eng, fn, reads=(), writes=()):
        return self._record(eng, fn, reads, writes, False)

    def dma(self, eng, out, in_, reads=(), writes=(), **kw):
        def fn(e):
            return e.dma_start(out=out, in_=in_, **kw)
        return self._record(eng, fn, reads, writes, True)

    def emit(self):
        nc = self.nc
        st = self.stack
        comp_sems = {}
        dma_sems = {}
        for e in self.ENGS:
            ne = (self.n_comp[e] + EPOCH - 1) // EPOCH
            comp_sems[e] = [st.enter_context(nc.semaphore(f"c_{e}_{i}")) for i in range(ne)]
            if self.n_dma[e]:
                dma_sems[e] = [st.enter_context(nc.semaphore(f"d_{e}_{i}")) for i in range(NSLOT)]
        ops = self.ops
        block = st.enter_context(nc.Block())

        def make(ename):
            def body(eng):
                seen_c = {e: -1 for e in self.ENGS}
                seen_d = {}
                my_dma_issued = 0
                for op_id in self.eng_ops[ename]:
                    op = ops[op_id]
                    need_c = {}
                    need_d = {}
                    if op.get("barrier") is not None:
                        sc, sd = op["barrier"]
                        for e2 in self.ENGS:
                            k2 = sc[e2] - 1
                            if k2 > seen_c[e2] and not (ename == e2):
                                eng.wait_ge(comp_sems[e2][k2 // EPOCH], (k2 % EPOCH) + 1)
                                seen_c[e2] = k2
                            if k2 >= 0 and ename == e2 and k2 > seen_c[e2]:
                                eng.wait_ge(comp_sems[e2][k2 // EPOCH], (k2 % EPOCH) + 1)
                                seen_c[e2] = k2
                            nd2 = sd[e2]
                            for slot in range(min(NSLOT, nd2)):
                                k3 = ((nd2 - 1 - slot) // NSLOT) * NSLOT + slot
                                if k3 > seen_d.get((e2, slot), -1):
                                    eng.wait_ge(dma_sems[e2][slot], 16 * (k3 // NSLOT + 1))
                                    seen_d[(e2, slot)] = k3
                        continue
                    for d in op["deps"]:
                        kind, e2, k2 = ops[d]["tok"]
                        if kind == "c":
                            if ename == "pe" and e2 == "pe":
                                continue
                            if k2 > seen_c[e2] and k2 > need_c.get(e2, -1):
                                need_c[e2] = k2
                        else:
                            key = (e2, k2 % NSLOT)
                            if k2 > seen_d.get(key, -1) and k2 > need_d.get(key, -1):
                                need_d[key] = k2
                    if op["is_dma"]:
                        k = op["tok"][2]
                        if k >= NSLOT:
                            key = (ename, k % NSLOT)
                            kk = k - NSLOT
                            if kk > seen_d.get(key, -1) and kk > need_d.get(key, -1):
                                need_d[key] = kk
                    for e2, k2 in need_c.items():
                        eng.wait_ge(comp_sems[e2][k2 // EPOCH], (k2 % EPOCH) + 1)
                        seen_c[e2] = k2
                    for (e2, slot), k2 in need_d.items():
                        eng.wait_ge(dma_sems[e2][slot], 16 * (k2 // NSLOT + 1))
                        seen_d[(e2, slot)] = k2
                    ins = op["fn"](eng)
                    kind, _, k = op["tok"]
                    if kind == "c":
                        ins.then_inc(comp_sems[ename][k // EPOCH], 1)
                    else:
                        ins.then_inc(dma_sems[ename][k % NSLOT], 16)
                nd = self.n_dma[ename]
                for slot in range(min(NSLOT, nd)):
                    cnt = (nd - 1 - slot) // NSLOT + 1
                    eng.wait_ge(dma_sems[ename][slot], 16 * cnt)
            return body

        block.sync(make("sync"))
        block.scalar(make("act"))
        block.vector(make("dve"))
        block.gpsimd(make("pool"))
        block.tensor(make("pe"))
        st.close()

D = 2048
NT = 18
TOK = 2304
EPS = 1e-6
TWO_PI = 2.0 * math.pi


class Arena:
    def __init__(self, P, nwords=49152):
        self.P = P
        self.base = P.sb("arena", [128, nwords], F32)
        self.off = 0
        self.cap = nwords * 4

    def alloc(self, name, shape, dtype, parts=128):
        esz = 2 if dtype == BF16 else 4
        n = 1
        for s in shape:
            n *= s
        nbytes = (n * esz + 31) // 32 * 32
        assert self.off + nbytes <= self.cap, (name, self.off, nbytes)
        ap = self.base.t[0:parts, self.off // 4:(self.off + nbytes) // 4]
        if dtype != F32:
            ap = ap.bitcast(dtype)
        ap = ap[:, 0:n]
        if len(shape) == 2:
            ap = ap.rearrange("p (a b) -> p a b", a=shape[0])
        elif len(shape) == 3:
            ap = ap.rearrange("p (a b c) -> p a b c", a=shape[0], b=shape[1])
        self.off += nbytes
        return T(ap, name)

    def mark(self):
        return self.off

    def release(self, m):
        self.off = m
        self.P.barrier()


def build_program(n_layers=4, dbg=None, unit=None, upto=99):
    nc = bass.Bass("TRN2", target_bir_lowering=False)
    P = Prog(nc)
    A = Arena(P)
    PS = P.ps("psall", [128, 4096], F32)
    PB = [Res(f"bank{b}") for b in range(8)]

    def pf(b, n=1):
        return PS.t[:, b * 512:(b + n) * 512]

    def pbf(b, n=1):
        return PS.t[:, b * 512:(b + n) * 512].bitcast(BF16)

    def inp(name, shape):
        return nc.dram_tensor(name, list(shape), F32, kind="ExternalInput").ap()

    x_in = inp("x_in", [2048, D] if unit is None else [1, 1]); ctx_in = inp("ctx_in", [256, D]); cT_in = inp("cT_in", [128, 32])
    mod_w = inp("mod_w", [4, D, 6 * D] if unit is None else [1, 1]); mod_b = inp("mod_b", [4, 6 * D]); norm_g = inp("norm_g", [8, D])
    ev_w_in = inp("ev_w_in", [2, D, 2560] if unit is None else [1, 1]); ev_w_out = inp("ev_w_out", [2, D, D] if unit is None else [1, 1])
    a_q_norm = inp("a_q_norm", [2, 128]); a_k_norm = inp("a_k_norm", [2, 128])
    s5_par = inp("s5_par", [2, 2, 128, 96])
    s5_bblk = inp("s5_bblk", [2, 2, 2, 32, 128, 128] if unit is None else [1, 1])
    s5_cblk = inp("s5_cblk", [2, 2, 2, 128, 32, 16] if unit is None else [1, 1])
    s5_d = inp("s5_d", [2, 1024]); s5_glu_w = inp("s5_glu_w", [2, 1024, 1024] if unit is None else [1, 1]); s5_glu_b = inp("s5_glu_b", [2, 1024])
    od_w_in = inp("od_w_in", [2, D, 3872] if unit is None else [1, 1]); od_w_out = inp("od_w_out", [2, D, D] if unit is None else [1, 1])
    c_sink = inp("c_sink", [2, 16]); m_conv_w = inp("m_conv_w", [2, 3, 1536]); m_conv_b = inp("m_conv_b", [2, 1536])
    m_dt_bias = inp("m_dt_bias", [2, 32]); m_a_log = inp("m_a_log", [2, 32]); m_d = inp("m_d", [2, 16]); m_norm_g = inp("m_norm_g", [2, 1024])
    moe_router = inp("moe_router", [4, D, 16] if unit is None else [1, 1]); moe_w_gate = inp("moe_w_gate", [4, 16, D, 1024] if unit is None else [1, 1])
    moe_w_up = inp("moe_w_up", [4, 16, D, 1024] if unit is None else [1, 1]); moe_w_down = inp("moe_w_down", [4, 16, 1024, D] if unit is None else [1, 1])
    final_norm_g = inp("final_norm_g", [1, D])
    k_ident = inp("k_ident", [128, 128]); k_iota = inp("k_iota", [128, 256]); k_iotap = inp("k_iotap", [128, 2])
    k_spos = inp("k_spos", [2, 128, TOK]); k_rope = inp("k_rope", [4, 2048, 64])
    k_maskw = inp("k_maskw", [128, 384]); k_tri = inp("k_tri", [4, 128, 128])
    y_out = nc.dram_tensor("y_out", [2048, D], F32, kind="ExternalOutput").ap()

    X = P.dram("X", [TOK, D], F32)
    MODV = P.dram("MODV", [2, 6 * D], F32)
    HT = P.dram("HT", [D, TOK], BF16)
    HTOK = P.dram("HTOK", [TOK, D], BF16)
    PROJ = P.dram("PROJ", [TOK, 3872], F32)
    MIX = P.dram("MIX", [TOK, D], BF16)
    MIXT = P.dram("MIXT", [D, TOK], BF16)
    ROUT = P.dram("ROUT", [3, 16, TOK], F32)
    YE = P.dram("YE", [16, 288, D], BF16)
    GT5 = P.dram("GT5", [TOK, 1024], BF16)
    GT5T = P.dram("GT5T", [1024, TOK], BF16)
    YF = P.dram("YF", [TOK, 1024], F32)
    XC = P.dram("XC", [TOK, 1536], BF16)
    DTD = P.dram("DTD", [TOK, 64], F32)
    SZ = P.dram("SZ", [TOK, 1024], F32)
    YB = P.dram("YB", [TOK, 1024], F32)

    def rows(i):
        return slice(i * 128, (i + 1) * 128)

    def mm(out, lhsT, rhs, start, stop, reads, writes):
        P.op("pe", lambda e: e.matmul(out, lhsT=lhsT, rhs=rhs, start=start, stop=stop), reads, writes)

    def tr(out, in_, ident, reads, writes):
        P.op("pe", lambda e: e.transpose(out, in_, ident), reads, writes)

    def act(out, in_, func, reads, writes, bias=None, scale=None, accum=None, eng="act"):
        kw = {}
        if bias is not None:
            kw["bias"] = bias
        if scale is not None:
            kw["scale"] = scale
        if accum is not None:
            kw["accum_out"] = accum
        P.op("act", lambda e: e.activation(out=out, in_=in_, func=func, **kw), reads, writes)

    def tt(eng, out, a, b, op, reads, writes):
        P.op(eng, lambda e: e.tensor_tensor(out=out, in0=a, in1=b, op=op), reads, writes)

    def ts(eng, out, a, s1, s2, op0, op1, reads, writes, accum=None):
        kw = {}
        if accum is not None:
            kw["accum_out"] = accum
        if op1 is None:
            P.op(eng, lambda e: e.tensor_scalar(out=out, in0=a, scalar1=s1, scalar2=None, op0=op0, **kw), reads, writes)
        else:
            P.op(eng, lambda e: e.tensor_scalar(out=out, in0=a, scalar1=s1, scalar2=s2, op0=op0, op1=op1, **kw), reads, writes)

    def stt(eng, out, a, s, b, op0, op1, reads, writes):
        P.op(eng, lambda e: e.scalar_tensor_tensor(out=out, in0=a, scalar=s, in1=b, op0=op0, op1=op1), reads, writes)

    def cp(eng, out, in_, reads, writes):
        if eng == "act":
            P.op("act", lambda e: e.copy(out=out, in_=in_), reads, writes)
        else:
            P.op(eng, lambda e: e.tensor_copy(out=out, in_=in_), reads, writes)

    def recip(out, in_, reads, writes):
        P.op("dve", lambda e: e.reciprocal(out=out, in_=in_), reads, writes)

    def gen(eng, name, reads, writes, **kw):
        P.op(eng, lambda e: getattr(e, name)(**kw), reads, writes)

    def bcast(ap, n=128):
        return ap.partition_broadcast(n)

    ident_bf = A.alloc("ident_bf", [128], BF16)
    ident_f = A.alloc("ident_f", [128], F32)
    cTs = A.alloc("cTs", [16, 2], F32)
    iotaS = A.alloc("iotaS", [256], F32)
    iotaP = A.alloc("iotaP", [2], F32)
    P.dma("pool", ident_bf[:], k_ident, writes=[ident_bf])
    P.dma("sync", ident_f[:], k_ident, writes=[ident_f])
    P.dma("sync", iotaS[:], k_iota, writes=[iotaS])
    P.dma("sync", iotaP[:], k_iotap, writes=[iotaP])
    P.dma("sync", cTs[:].rearrange("p a b -> p (a b)"), cT_in, writes=[cTs])
    act(cTs[:], cTs[:], AF.Silu, [cTs], [cTs])
    if unit is None:
        P.dma("sync", X[0:256, :], ctx_in, writes=[X.sub(0), X.sub(1)])
        for i in range(16):
            P.dma("sync", X[rows(i + 2), :], x_in[rows(i), :], writes=[X.sub(i + 2)])

    def stage_mod(l):
        m = A.mark()
        wbufs = [A.alloc(f"modw{i}", [2048], F32) for i in range(3)]
        bb = A.alloc("modbb", [2048], F32, parts=2)
        ot = A.alloc("modo", [2048], F32, parts=2)
        n = 0
        for cg in range(6):
            c0 = cg * 2048
            for k in range(16):
                wb = wbufs[n % 3]; n += 1
                P.dma("sync", wb[:], mod_w[l, k * 128:(k + 1) * 128, c0:c0 + 2048], writes=[wb])
                for q in range(4):
                    mm(pf(q)[0:2, :], cTs[:, k, :], wb[:, q * 512:(q + 1) * 512], k == 0, k == 15, [wb, cTs], [PB[q]])
            P.dma("sync", bb[:], bcast(mod_b[l:l + 1, c0:c0 + 2048], 2), writes=[bb])
            tt("dve", ot[:], pf(0, 4)[0:2, :], bb[:], ALU.add, [PB[0], PB[1], PB[2], PB[3], bb], [ot])
            P.dma("pool", MODV[:, c0:c0 + 2048], ot[:], reads=[ot], writes=[MODV])
        A.release(m)

    def stage_norm(g_row, sh_idx, sc_idx, dstT, dst_tok, tiles=range(NT), final_out=None):
        m = A.mark()
        G = [A.alloc(f"nG{s}", [2048], F32) for s in range(2)]
        S = [A.alloc(f"nS{s}", [2048], F32) for s in range(2)]
        gt = A.alloc("ngt", [2048], F32)
        tmp = A.alloc("ntmp", [2048], F32)
        P.dma("sync", gt[:], bcast(g_row), writes=[gt])
        if final_out is None:
            for s in range(2):
                r = 1 if s == 0 else 0
                P.dma("sync", tmp[:], bcast(MODV[r:r + 1, sc_idx * 2048:(sc_idx + 1) * 2048]), reads=[MODV], writes=[tmp])
                stt("dve", G[s][:], tmp[:], 1.0, gt[:], ALU.add, ALU.mult, [tmp, gt], [G[s]])
                P.dma("sync", S[s][:], bcast(MODV[r:r + 1, sh_idx * 2048:(sh_idx + 1) * 2048]), reads=[MODV], writes=[S[s]])
        xts = [A.alloc(f"nx{i}", [2048], F32) for i in range(2)]
        junk = A.alloc("njunk", [2048], F32)
        hn = A.alloc("nhn", [2048], F32)
        hbs = [A.alloc(f"nhb{i}", [2048], BF16) for i in range(2)]
        hTs = [A.alloc(f"nhT{i}", [16, 128], BF16) for i in range(2)]
        sss = [A.alloc(f"nss{i}", [2], F32) for i in range(2)]
        dstT_v = dstT.t.rearrange("(k p) t -> p k t", p=128) if dstT is not None else None
        for n, i in enumerate(tiles):
            s = 0 if i < 2 else 1
            xt = xts[n % 2]; hb = hbs[n % 2]; hT = hTs[n % 2]; ss = sss[n % 2]
            P.dma("sync", xt[:], X[rows(i), :], reads=[X.sub(i)], writes=[xt])
            act(junk[:], xt[:], AF.Square, [xt], [junk, ss], accum=ss[:, 0:1])
            ts("dve", ss[:, 1:2], ss[:, 0:1], 1.0 / D, EPS, ALU.mult, ALU.add, [ss], [ss])
            act(ss[:, 1:2], ss[:, 1:2], AF.Sqrt, [ss], [ss])
            recip(ss[:, 1:2], ss[:, 1:2], [ss], [ss])
            if final_out is not None:
                stt("dve", hn[:], xt[:], ss[:, 1:2], gt[:], ALU.mult, ALU.mult, [xt, ss, gt], [hn])
                P.dma("pool", final_out[rows(i - 2), :], hn[:], reads=[hn])
                continue
            stt("dve", hn[:], xt[:], ss[:, 1:2], G[s][:], ALU.mult, ALU.mult, [xt, ss, G[s]], [hn])
            tt("pool", hb[:], hn[:], S[s][:], ALU.add, [hn, S[s]], [hb])
            if dst_tok is not None:
                P.dma("pool", dst_tok[rows(i), :], hb[:], reads=[hb], writes=[dst_tok.sub(i)])
            for q in range(2):
                bk = 6 + q
                for jj in range(8):
                    k = q * 8 + jj
                    tr(pbf(bk)[:, jj * 128:(jj + 1) * 128], hb[:, k * 128:(k + 1) * 128], ident_bf[:], [hb, ident_bf], [PB[bk]])
                cp("act" if q == 0 else "dve", hT[:, q * 8:(q + 1) * 8, :], pbf(bk).rearrange("p (a b) -> p a b", a=8), [PB[bk]], [hT])
            P.dma("pool", dstT_v[:, :, rows(i)], hT[:], reads=[hT], writes=[dstT.sub(i)])
        A.release(m)

    def stage_transpose(SRC, DST, ncol):
        m = A.mark()
        kt = ncol // 128
        tbs = [A.alloc(f"tb{i}", [ncol], BF16) for i in range(2)]
        tTs = [A.alloc(f"tT{i}", [kt, 128], BF16) for i in range(2)]
        dv = DST.t.rearrange("(k p) t -> p k t", p=128)
        for i in range(NT):
            tb = tbs[i % 2]; tT = tTs[i % 2]
            P.dma("sync", tb[:], SRC[rows(i), 0:ncol], reads=[SRC], writes=[tb])
            for q in range(kt // 8):
                bk = 6 + (q % 2)
                for jj in range(8):
                    k = q * 8 + jj
                    tr(pbf(bk)[:, jj * 128:(jj + 1) * 128], tb[:, k * 128:(k + 1) * 128], ident_bf[:], [tb, ident_bf], [PB[bk]])
                cp("act" if q % 2 == 0 else "dve", tT[:, q * 8:(q + 1) * 8, :], pbf(bk).rearrange("p (a b) -> p a b", a=8), [PB[bk]], [tT])
            P.dma("pool", dv[:, 0:kt, rows(i)], tT[:], reads=[tT], writes=[DST.sub(i)])
        A.release(m)

    def stage_linear(srcT, KT, W, N, epi_factory, GW=1024):
        m = A.mark()
        wbs = [A.alloc(f"lw{i}", [KT, GW], BF16) for i in range(2)]
        hts = [A.alloc(f"lh{i}", [KT, 128], BF16) for i in range(3)]
        epi = epi_factory()
        sv = srcT.t.rearrange("(k p) t -> p k t", p=128)
        ng = (N + GW - 1) // GW
        cnt = 0
        for gi in range(ng):
            c0 = gi * GW
            gw = min(GW, N - c0)
            wb = wbs[gi % 2]
            for kq in range(0, KT, 4):
                P.dma("pool", wb[:, kq:kq + 4, 0:gw], W[kq * 128:(kq + 4) * 128, c0:c0 + gw].rearrange("(k p) n -> p k n", p=128), writes=[wb])
            for i in range(NT):
                ht = hts[cnt % 3]
                pbase = 0 if cnt % 2 == 0 else 2
                cnt += 1
                P.dma("sync", ht[:], sv[:, 0:KT, rows(i)], reads=[srcT.sub(i)], writes=[ht])
                nch = (gw + 511) // 512
                for ch in range(nch):
                    cw = min(512, gw - ch * 512)
                    for k in range(KT):
                        mm(pf(pbase + ch)[:, 0:cw], ht[:, k, :], wb[:, k, ch * 512:ch * 512 + cw], k == 0, k == KT - 1, [ht, wb], [PB[pbase + ch]])
                epi(i, c0, gw, pf(pbase, 2)[:, 0:gw], [PB[pbase], PB[pbase + 1]])
        A.release(m)

    def epi_store(DST, col_off=0):
        def factory():
            obs = [A.alloc(f"eo{i}", [1024], F32) for i in range(2)]
            st = {"n": 0}

            def epi(i, c0, gw, psap, pres):
                ob = obs[st["n"] % 2]; st["n"] += 1
                cp("act", ob[:, 0:gw], psap, pres, [ob])
                P.dma("pool", DST[rows(i), col_off + c0:col_off + c0 + gw], ob[:, 0:gw], reads=[ob], writes=[DST])
            return epi
        return factory

    def load_gate(gate_idx):
        GT = [A.alloc(f"eG{s}", [2048], F32) for s in range(2)]
        for s in range(2):
            r = 1 if s == 0 else 0
            P.dma("sync", GT[s][:], bcast(MODV[r:r + 1, gate_idx * 2048:(gate_idx + 1) * 2048]), reads=[MODV], writes=[GT[s]])
        return GT

    def epi_resid(gate_idx):
        def factory():
            GT = load_gate(gate_idx)
            xbs = [A.alloc(f"ex{i}", [1024], F32) for i in range(2)]
            tbs = [A.alloc(f"et{i}", [1024], F32) for i in range(2)]
            st = {"n": 0}

            def epi(i, c0, gw, psap, pres):
                s = 0 if i < 2 else 1
                xb = xbs[st["n"] % 2]; tb = tbs[st["n"] % 2]; st["n"] += 1
                P.dma("sync", xb[:, 0:gw], X[rows(i), c0:c0 + gw], reads=[X.sub(i)], writes=[xb])
                tt("dve", tb[:, 0:gw], psap, GT[s][:, c0:c0 + gw], ALU.mult, pres + [GT[s]], [tb])
                tt("pool", xb[:, 0:gw], xb[:, 0:gw], tb[:, 0:gw], ALU.add, [xb, tb], [xb])
                P.dma("pool", X[rows(i), c0:c0 + gw], xb[:, 0:gw], reads=[xb], writes=[X.sub(i)])
            return epi
        return factory

    STREAMS = [(0, 2, 32), (2, 16, 256)]
    YE_OFF = [0, 32]

    def stage_moe(l):
        m = A.mark()
        wr = A.alloc("wr", [16, 16], BF16)
        P.dma("pool", wr[:], moe_router[l].rearrange("(k p) e -> p k e", p=128), writes=[wr])
        hts = [A.alloc(f"rh{i}", [16, 128], BF16) for i in range(2)]
        aff = A.alloc("aff", [16], F32)
        sm = A.alloc("rsm", [4], F32)
        affT = A.alloc("affT", [TOK], F32, parts=16)
        work = A.alloc("rwork", [2048], F32, parts=16)
        m8 = A.alloc("m8", [8], F32, parts=16)
        maskT = A.alloc("maskT", [TOK], F32, parts=16)
        gateT = A.alloc("gateT", [TOK], F32, parts=16)
        posT = A.alloc("posT", [TOK], F32, parts=16)
        ones = A.alloc("rones", [2048], F32, parts=16)
        P.op("dve", lambda e: e.memset(ones[:], 1.0), [], [ones])
        sv = HT.t.rearrange("(k p) t -> p k t", p=128)
        for i in range(NT):
            ht = hts[i % 2]
            P.dma("sync", ht[:], sv[:, :, rows(i)], reads=[HT.sub(i)], writes=[ht])
            for k in range(16):
                mm(pf(4)[:, 0:16], ht[:, k, :], wr[:, k, :], k == 0, k == 15, [ht, wr], [PB[4]])
            P.op("dve", lambda e: e.reduce_max(out=sm[:, 0:1], in_=pf(4)[:, 0:16], axis=AX.X), [PB[4]], [sm])
            ts("dve", sm[:, 1:2], sm[:, 0:1], -1.0, None, ALU.mult, None, [sm], [sm])
            act(aff[:], pf(4)[:, 0:16], AF.Exp, [PB[4], sm], [aff, sm], bias=sm[:, 1:2], accum=sm[:, 2:3])
            recip(sm[:, 3:4], sm[:, 2:3], [sm], [sm])
            ts("dve", aff[:], aff[:], sm[:, 3:4], None, ALU.mult, None, [aff, sm], [aff])
            tr(pf(5)[0:16, 0:128], aff[:, 0:16], ident_f[:], [aff, ident_f], [PB[5]])
            cp("act", affT[:, rows(i)], pf(5)[0:16, 0:128], [PB[5]], [affT])
        for (t0, ntl, cap) in STREAMS:
            c0 = t0 * 128; Tn = ntl * 128
            cp("dve", work[:, 0:Tn], affT[:, c0:c0 + Tn], [affT], [work])
            for r in range(cap // 8):
                gen("dve", "max", [work], [m8], out=m8[:], in_=work[:, 0:Tn])
                if r < cap // 8 - 1:
                    gen("dve", "match_replace", [work, m8], [work], out=work[:, 0:Tn], in_to_replace=m8[:], in_values=work[:, 0:Tn], imm_value=-1.0)
            ts("dve", maskT[:, c0:c0 + Tn], affT[:, c0:c0 + Tn], m8[:, 7:8], None, ALU.is_ge, None, [affT, m8], [maskT])
            tt("dve", gateT[:, c0:c0 + Tn], affT[:, c0:c0 + Tn], maskT[:, c0:c0 + Tn], ALU.mult, [affT, maskT], [gateT])
            gen("dve", "tensor_tensor_scan", [ones, maskT], [posT], out=posT[:, c0:c0 + Tn], data0=ones[:, 0:Tn], data1=maskT[:, c0:c0 + Tn], initial=0.0, op0=ALU.mult, op1=ALU.add)
            ts("dve", posT[:, c0:c0 + Tn], posT[:, c0:c0 + Tn], -1.0, None, ALU.add, None, [posT], [posT])
        P.dma("pool", ROUT[0], posT[:], reads=[posT], writes=[ROUT])
        P.dma("pool", ROUT[1], gateT[:], reads=[gateT], writes=[ROUT])
        m_keep = A.mark()
        A.off = m
        A.P.barrier()
        postok = A.alloc("postok", [NT, 16], F32)
        masktok = A.alloc("masktok", [NT, 16], F32)
        for i in range(NT):
            tr(pf(5)[:, 0:16], posT[:, rows(i)], ident_f[0:16, 0:16], [posT, ident_f], [PB[5]])
            cp("act", postok[:, i, :], pf(5)[:, 0:16], [PB[5]], [postok])
            tr(pf(4)[:, 0:16], maskT[:, rows(i)], ident_f[0:16, 0:16], [maskT, ident_f], [PB[4]])
            cp("dve", masktok[:, i, :], pf(4)[:, 0:16], [PB[4]], [masktok])
        A.P.barrier()
        hkb = [A.alloc(f"hk{i}", [16, 512], BF16) for i in range(2)]
        ring = [A.alloc(f"wring{i}", [8192], BF16) for i in range(6)]
        sel = A.alloc("sel", [16, 256], BF16)
        xsT = A.alloc("xsT", [16, 256], BF16)
        hidT = A.alloc("hidT", [8, 256], BF16)
        stmp = A.alloc("stmp", [256], F32)
        yebs = [A.alloc(f"yeb{i}", [2048], BF16) for i in range(2)]
        rn = 0
        yn = 0
        for e in range(16):
            wg = []; wu = []; wd = []
            for h2 in range(2):
                b = ring[rn % 6]; rn += 1
                P.dma("pool", b[:].rearrange("p (k n) -> p k n", k=16), moe_w_gate[l, e, :, h2 * 512:(h2 + 1) * 512].rearrange("(k p) n -> p k n", p=128), writes=[b])
                wg.append(b)
                b = ring[rn % 6]; rn += 1
                P.dma("pool", b[:].rearrange("p (k n) -> p k n", k=16), moe_w_up[l, e, :, h2 * 512:(h2 + 1) * 512].rearrange("(k p) n -> p k n", p=128), writes=[b])
                wu.append(b)
            for h2 in range(2):
                b = ring[rn % 6]; rn += 1
                P.dma("pool", b[:].rearrange("p (k n) -> p k n", k=4), moe_w_down[l, e, h2 * 512:(h2 + 1) * 512, :].rearrange("(k p) n -> p k n", p=128), writes=[b])
                wd.append(b)
            for si, (t0, ntl, cap) in enumerate(STREAMS):
                S_ = cap
                SP = min(S_, 128)
                SH = (S_ + 127) // 128
                for tl in range(ntl):
                    ts("dve", sel[:, tl, 0:S_], iotaS[:, 0:S_], postok[:, t0 + tl, e:e + 1], masktok[:, t0 + tl, e:e + 1], ALU.is_equal, ALU.mult, [iotaS, postok, masktok], [sel])
                for dq in range(4):
                    hk = hkb[dq % 2]
                    P.dma("sync", hk[:, 0:ntl, :], HTOK[t0 * 128:(t0 + ntl) * 128, dq * 512:(dq + 1) * 512].rearrange("(n p) c -> p n c", p=128),
                          reads=[HTOK.sub(t0 + j) for j in range(ntl)], writes=[hk])
                    for dl in range(4):
                        dt_ = dq * 4 + dl
                        bk = dl % 2
                        for tl in range(ntl):
                            mm(pf(bk)[:, 0:S_], hk[:, tl, dl * 128:(dl + 1) * 128], sel[:, tl, 0:S_], tl == 0, tl == ntl - 1, [hk, sel], [PB[bk]])
                        cp("act" if dl % 2 == 0 else "dve", xsT[:, dt_, 0:S_], pf(bk)[:, 0:S_], [PB[bk]], [xsT])
                for f in range(8):
                    h2 = f // 4; fl = f % 4
                    wgv = wg[h2][:].rearrange("p (k n) -> p k n", k=16)
                    wuv = wu[h2][:].rearrange("p (k n) -> p k n", k=16)
                    for k in range(16):
                        mm(pf(2)[:, 0:S_], wgv[:, k, fl * 128:(fl + 1) * 128], xsT[:, k, 0:S_], k == 0, k == 15, [wg[h2], xsT], [PB[2]])
                    for k in range(16):
                        mm(pf(3)[:, 0:S_], wuv[:, k, fl * 128:(fl + 1) * 128], xsT[:, k, 0:S_], k == 0, k == 15, [wu[h2], xsT], [PB[3]])
                    act(stmp[:, 0:S_], pf(2)[:, 0:S_], AF.Silu, [PB[2]], [stmp])
                    tt("dve", hidT[:, f, 0:S_], stmp[:, 0:S_], pf(3)[:, 0:S_], ALU.mult, [stmp, PB[3]], [hidT])
                for sh in range(SH):
                    yeb = yebs[yn % 2]; yn += 1
                    for dc in range(4):
                        bk = 4 + dc % 2
                        for f in range(8):
                            wdv = wd[f // 4][:].rearrange("p (k n) -> p k n", k=4)
                            mm(pf(bk)[0:SP, :], hidT[:, f, sh * 128:sh * 128 + SP], wdv[:, f % 4, dc * 512:(dc + 1) * 512], f == 0, f == 7, [hidT, wd[f // 4]], [PB[bk]])
                        cp("act" if dc % 2 == 0 else "dve", yeb[0:SP, dc * 512:(dc + 1) * 512], pf(bk)[0:SP, :], [PB[bk]], [yeb])
                    r0 = YE_OFF[si] + sh * 128
                    P.dma("sync", YE[e, r0:r0 + SP, :], yeb[0:SP, :], reads=[yeb], writes=[YE])
        A.release(m)
        m = A.mark()
        GT = load_gate(5)
        for si, (t0, ntl, cap) in enumerate(STREAMS):
            m2 = A.mark()
            S_ = cap
            SP = min(S_, 128)
            SH = (S_ + 127) // 128
            yall = A.alloc("yall", [16 * SH, 2048], BF16, parts=SP)
            for e in range(16):
                P.dma("sync", yall[:, e * SH:(e + 1) * SH, :], YE[e, YE_OFF[si]:YE_OFF[si] + S_, :].rearrange("(h p) d -> p h d", p=SP), reads=[YE], writes=[yall])
            posb = A.alloc("posb", [16, 128], F32)
            gateb = A.alloc("gateb", [16, 128], F32)
            tmpf = A.alloc("tmpf", [16, 128], F32)
            stg = [A.alloc(f"stg{h}", [16, 128], BF16) for h in range(SH)]
            xbs = [A.alloc(f"sx{i}", [512], F32) for i in range(2)]
            tbs = [A.alloc(f"st{i}", [512], F32) for i in range(2)]
            n = 0
            for tl in range(ntl):
                i = t0 + tl
                s = 0 if i < 2 else 1
                P.dma("sync", posb[0:SP], bcast(ROUT[0:1, :, rows(i)], SP), reads=[ROUT], writes=[posb])
                P.dma("sync", gateb[0:SP], bcast(ROUT[1:2, :, rows(i)], SP), reads=[ROUT], writes=[gateb])
                for sh in range(SH):
                    ts("dve", tmpf[0:SP], posb[0:SP], iotaP[0:SP, sh:sh + 1], None, ALU.is_equal, None, [posb, iotaP], [tmpf])
                    tt("dve", stg[sh][0:SP], tmpf[0:SP], gateb[0:SP], ALU.mult, [tmpf, gateb], [stg[sh]])
                for dc in range(4):
                    bk = dc % 2
                    tot = 16 * SH
                    q = 0
                    for e in range(16):
                        for sh in range(SH):
                            mm(pf(bk)[:, :], stg[sh][0:SP, e, :], yall[0:SP, e * SH + sh, dc * 512:(dc + 1) * 512], q == 0, q == tot - 1, [stg[sh], yall], [PB[bk]])
                            q += 1
                    xb = xbs[n % 2]; tb = tbs[n % 2]; n += 1
                    P.dma("sync", xb[:], X[rows(i), dc * 512:(dc + 1) * 512], reads=[X.sub(i)], writes=[xb])
                    tt("dve", tb[:], pf(bk), GT[s][:, dc * 512:(dc + 1) * 512], ALU.mult, [PB[bk], GT[s]], [tb])
                    tt("pool", xb[:], xb[:], tb[:], ALU.add, [xb, tb], [xb])
                    P.dma("pool", X[rows(i), dc * 512:(dc + 1) * 512], xb[:], reads=[xb], writes=[X.sub(i)])
            A.release(m2)
        A.release(m)

    def stage_attn_a(li):
        m = A.mark()
        qT = A.alloc("qT", [8, TOK], BF16)
        kT = A.alloc("kT", [2, TOK], BF16)
        vA = A.alloc("vA", [NT, 256], BF16)
        gq = A.alloc("gq", [128], F32); gk = A.alloc("gk", [128], F32)
        P.dma("sync", gq[:], bcast(a_q_norm[li:li + 1, :]), writes=[gq])
        P.dma("sync", gk[:], bcast(a_k_norm[li:li + 1, :]), writes=[gk])
        m1 = A.mark()
        qks = [A.alloc(f"qk{i}", [10, 128], F32) for i in range(2)]
        sq = A.alloc("qsq", [10, 128], F32)
        qn = A.alloc("qn", [10, 128], F32)
        qb = A.alloc("qb", [10, 128], BF16)
        ssq = A.alloc("ssq", [10], F32)
        cs = [A.alloc(f"cs{i}", [2, 64], F32) for i in range(2)]
        t1 = A.alloc("rt1", [10, 64], F32); t2 = A.alloc("rt2", [10, 64], F32)
        vf = [A.alloc(f"vf{i}", [256], F32) for i in range(2)]
        for i in range(NT):
            qk = qks[i % 2]
            P.dma("sync", qk[:].rearrange("p a b -> p (a b)"), PROJ[rows(i), 0:1280], reads=[PROJ], writes=[qk])
            P.dma("sync", vf[i % 2][:], PROJ[rows(i), 1280:1536], reads=[PROJ], writes=[vf[i % 2]])
            cp("pool", vA[:, i, :], vf[i % 2][:], [vf[i % 2]], [vA])
            tt("pool", sq[:], qk[:], qk[:], ALU.mult, [qk], [sq])
            P.op("dve", lambda e: e.tensor_reduce(out=ssq[:], in_=sq[:], axis=AX.X, op=ALU.add), [sq], [ssq])
            ts("dve", ssq[:], ssq[:], 1.0 / 128, EPS, ALU.mult, ALU.add, [ssq], [ssq])
            act(ssq[:], ssq[:], AF.Sqrt, [ssq], [ssq])
            recip(ssq[:], ssq[:], [ssq], [ssq])
            tt("dve", qn[:], qk[:], ssq[:].unsqueeze(2).to_broadcast([128, 10, 128]), ALU.mult, [qk, ssq], [qn])
            tt("dve", qn[:, 0:8, :], qn[:, 0:8, :], gq[:].unsqueeze(1).to_broadcast([128, 8, 128]), ALU.mult, [qn, gq], [qn])
            tt("dve", qn[:, 8:10, :], qn[:, 8:10, :], gk[:].unsqueeze(1).to_broadcast([128, 2, 128]), ALU.mult, [qn, gk], [qn])
            if i >= 2:
                c = cs[i % 2]
                P.dma("sync", c[:, 0, :], k_rope[0, rows(i - 2), :], writes=[c])
                P.dma("sync", c[:, 1, :], k_rope[1, rows(i - 2), :], writes=[c])
                cb = c[:, 0, :].unsqueeze(1).to_broadcast([128, 10, 64])
                sb = c[:, 1, :].unsqueeze(1).to_broadcast([128, 10, 64])
                x1 = qn[:, :, 0:64]; x2 = qn[:, :, 64:128]
                tt("dve", t1[:], x1, cb, ALU.mult, [qn, c], [t1])
                tt("pool", t2[:], x2, sb, ALU.mult, [qn, c], [t2])
                tt("dve", qb[:, :, 0:64], t1[:], t2[:], ALU.subtract, [t1, t2], [qb])
                tt("dve", t1[:], x1, sb, ALU.mult, [qn, c, qb], [t1])
                tt("pool", t2[:], x2, cb, ALU.mult, [qn, c, qb], [t2])
                tt("dve", qb[:, :, 64:128], t1[:], t2[:], ALU.add, [t1, t2], [qb])
            else:
                cp("dve", qb[:], qn[:], [qn], [qb])
            for q in range(2):
                bk = 6 + q
                nb = 8 if q == 0 else 2
                for jj in range(nb):
                    h = q * 8 + jj
                    tr(pbf(bk)[:, jj * 128:(jj + 1) * 128], qb[:, h, :], ident_bf[:], [qb, ident_bf], [PB[bk]])
                if q == 0:
                    cp("act", qT[:, :, rows(i)], pbf(bk).rearrange("p (a b) -> p a b", a=8), [PB[bk]], [qT])
                else:
                    cp("dve", kT[:, :, rows(i)], pbf(bk)[:, 0:256].rearrange("p (a b) -> p a b", a=2), [PB[bk]], [kT])
        A.release(m1)
        pbs = [A.alloc(f"pb{i}", [TOK], BF16) for i in range(2)]
        pT = A.alloc("pT", [NT, 128], BF16)
        sm = A.alloc("asm", [4], F32)
        omix = [A.alloc(f"omix{i}", [1024], BF16) for i in range(2)]
        scale = 128 ** -0.5
        n = 0
        for i in range(NT):
            nk = 256 if i < 2 else TOK
            nkt = nk // 128
            om = omix[i % 2]
            for h in range(8):
                g = h // 4
                pb = pbs[n % 2]; n += 1
                nch = (nk + 511) // 512
                for ch in range(nch):
                    cw = min(512, nk - ch * 512)
                    mm(pf(ch)[:, 0:cw], qT[:, h, rows(i)], kT[:, g, ch * 512:ch * 512 + cw], True, True, [qT, kT], [PB[ch]])
                sres = [PB[ch] for ch in range(nch)]
                gen("dve", "reduce_max", sres, [sm], out=sm[:, 0:1], in_=pf(0, 5)[:, 0:nk], axis=AX.X)
                ts("dve", sm[:, 1:2], sm[:, 0:1], -scale, None, ALU.mult, None, [sm], [sm])
                act(pb[:, 0:nk], pf(0, 5)[:, 0:nk], AF.Exp, sres + [sm], [pb, sm], bias=sm[:, 1:2], scale=scale, accum=sm[:, 2:3])
                recip(sm[:, 3:4], sm[:, 2:3], [sm], [sm])
                for q in range((nkt + 7) // 8):
                    bk = 6 + q % 2
                    nb = min(8, nkt - q * 8)
                    for jj in range(nb):
                        kt_ = q * 8 + jj
                        tr(pbf(bk)[:, jj * 128:(jj + 1) * 128], pb[:, kt_ * 128:(kt_ + 1) * 128], ident_bf[:], [pb, ident_bf], [PB[bk]])
                    cp("act" if q % 2 == 0 else "dve", pT[:, q * 8:q * 8 + nb, :], pbf(bk)[:, 0:nb * 128].rearrange("p (a b) -> p a b", a=nb), [PB[bk]], [pT])
                for kt_ in range(nkt):
                    mm(pf(5)[:, 0:128], pT[:, kt_, :], vA[:, kt_, g * 128:(g + 1) * 128], kt_ == 0, kt_ == nkt - 1, [pT, vA], [PB[5]])
                act(om[:, h * 128:(h + 1) * 128], pf(5)[:, 0:128], AF.Copy, [PB[5], sm], [om], scale=sm[:, 3:4])
            P.dma("pool", MIX[rows(i), 0:1024], om[:], reads=[om], writes=[MIX])
        A.release(m)

    def stage_s5(li):
        m = A.mark()
        uT = A.alloc("uT", [8, TOK], BF16)
        m1 = A.mark()
        ufs = [A.alloc(f"uf{i}", [1024], F32) for i in range(2)]
        ubs = [A.alloc(f"ub{i}", [1024], BF16) for i in range(2)]
        for i in range(NT):
            uf = ufs[i % 2]; ub = ubs[i % 2]
            P.dma("sync", uf[:], PROJ[rows(i), 1536:2560], reads=[PROJ], writes=[uf])
            cp("pool", ub[:], uf[:], [uf], [ub])
            for jj in range(8):
                tr(pbf(6 + i % 2)[:, jj * 128:(jj + 1) * 128], ub[:, jj * 128:(jj + 1) * 128], ident_bf[:], [ub, ident_bf], [PB[6 + i % 2]])
            cp("act", uT[:, :, rows(i)], pbf(6 + i % 2).rearrange("p (a b) -> p a b", a=8), [PB[6 + i % 2]], [uT])
        A.release(m1)
        par = [A.alloc(f"s5par{d}", [3, 32], F32) for d in range(2)]
        rr = [A.alloc(f"s5r{d}", [32], F32) for d in range(2)]
        th = [A.alloc(f"s5th{d}", [32], F32) for d in range(2)]
        cr = [A.alloc(f"s5cr{d}", [32], F32) for d in range(2)]
        ci = [A.alloc(f"s5ci{d}", [32], F32) for d in range(2)]
        pt = [A.alloc(f"s5pt{i}", [32], F32) for i in range(8)]
        pti = A.alloc("s5pti", [32], I32)

        def sincos(dst_sin, dst_cos, ang, wid, rres, tmpa, tmpk, tmpi):
            for (dst, shift) in ((dst_sin, 0.0), (dst_cos, math.pi / 2)):
                ts("dve", tmpa, ang, shift, 1.0 / TWO_PI, ALU.add, ALU.mult, rres, rres)
                cp("dve", tmpi, tmpa, rres, rres)
                cp("dve", tmpk, tmpi, rres, rres)
                stt("dve", tmpa, tmpk, -1.0, tmpa, ALU.mult, ALU.add, rres, rres)
                ts("dve", tmpa, tmpa, TWO_PI, math.pi, ALU.mult, ALU.min, rres, rres)
                ts("dve", tmpa, tmpa, -math.pi, None, ALU.max, None, rres, rres)
                act(dst, tmpa, AF.Sin, rres, rres)

        for d in range(2):
            R_ = [par[d], rr[d], th[d], cr[d], ci[d]] + pt + [pti]
            P.dma("sync", par[d][:].rearrange("p a b -> p (a b)"), s5_par[li, d], writes=[par[d]])
            are = par[d][:, 0, :]; aim = par[d][:, 1, :]
            dtv = pt[0][:]
            act(dtv, par[d][:, 2, :], AF.Exp, R_, R_)
            tt("dve", pt[1][:], are, dtv, ALU.mult, R_, R_)
            act(rr[d][:], pt[1][:], AF.Exp, R_, R_)
            tt("dve", th[d][:], aim, dtv, ALU.mult, R_, R_)
            sincos(pt[2][:], pt[3][:], th[d][:], 32, R_, pt[4][:], pt[5][:], pti[:])
            tt("dve", pt[4][:], rr[d][:], pt[3][:], ALU.mult, R_, R_)
            ts("dve", pt[4][:], pt[4][:], -1.0, None, ALU.add, None, R_, R_)
            tt("dve", pt[5][:], rr[d][:], pt[2][:], ALU.mult, R_, R_)
            tt("dve", pt[6][:], are, are, ALU.mult, R_, R_)
            tt("dve", pt[7][:], aim, aim, ALU.mult, R_, R_)
            tt("dve", pt[6][:], pt[6][:], pt[7][:], ALU.add, R_, R_)
            recip(pt[6][:], pt[6][:], R_, R_)
            tt("dve", pt[7][:], pt[4][:], are, ALU.mult, R_, R_)
            tt("dve", pt[1][:], pt[5][:], aim, ALU.mult, R_, R_)
            tt("dve", pt[7][:], pt[7][:], pt[1][:], ALU.add, R_, R_)
            tt("dve", cr[d][:], pt[7][:], pt[6][:], ALU.mult, R_, R_)
            tt("dve", pt[7][:], pt[5][:], are, ALU.mult, R_, R_)
            tt("dve", pt[1][:], pt[4][:], aim, ALU.mult, R_, R_)
            tt("dve", pt[7][:], pt[7][:], pt[1][:], ALU.subtract, R_, R_)
            tt("dve", ci[d][:], pt[7][:], pt[6][:], ALU.mult, R_, R_)
        spos = [A.alloc(f"spos{d}", [TOK], F32) for d in range(2)]
        for d in range(2):
            P.dma("sync", spos[d][:], k_spos[d], writes=[spos[d]])
        bw = [A.alloc(f"s5bw{i}", [2, 128], BF16) for i in range(2)]
        cw_ = [A.alloc(f"s5cw{i}", [2, 16], F32) for i in range(2)]
        cblk = [A.alloc(f"s5cb{i}", [2, 2, 32], BF16) for i in range(2)]
        ctmp = A.alloc("s5ct", [4, 16], F32)
        W = [A.alloc(f"s5w{i}", [TOK], F32) for i in range(8)]
        Wi = A.alloc("s5wi", [TOK], I32)
        hb = [A.alloc(f"s5h{i}", [TOK], BF16) for i in range(4)]
        ysb = A.alloc("s5y", [NT, 32], F32)
        bn = 0
        for j in range(32):
            cb = cblk[j % 2]
            gen("pool", "memset", [], [cb], ap=cb[:], constant=0.0)
            for d in range(2):
                b_ = bw[bn % 2]; c_ = cw_[bn % 2]; bn += 1
                P.dma("pool", b_[:, 0, :], s5_bblk[li, d, 0, j], writes=[b_])
                P.dma("pool", b_[:, 1, :], s5_bblk[li, d, 1, j], writes=[b_])
                P.dma("sync", c_[:, 0, :], s5_cblk[li, d, 0, :, j, :], writes=[c_])
                P.dma("sync", c_[:, 1, :], s5_cblk[li, d, 1, :, j, :], writes=[c_])
                crj = cr[d][:, j:j + 1]; cij = ci[d][:, j:j + 1]
                ts("dve", ctmp[:, 0, :], c_[:, 0, :], crj, None, ALU.mult, None, [c_, cr[d]], [ctmp])
                ts("dve", ctmp[:, 1, :], c_[:, 1, :], cij, None, ALU.mult, None, [c_, ci[d]], [ctmp])
                ts("dve", ctmp[:, 2, :], c_[:, 0, :], cij, None, ALU.mult, None, [c_, ci[d]], [ctmp])
                ts("dve", ctmp[:, 3, :], c_[:, 1, :], crj, None, ALU.mult, None, [c_, cr[d]], [ctmp])
                for q in range(2):
                    ps_ = slice(q * 64, (q + 1) * 64)
                    tt("dve", cb[ps_, d, 0, q * 16:(q + 1) * 16], ctmp[ps_, 0, :], ctmp[ps_, 1, :], ALU.subtract, [ctmp], [cb])
                    stt("dve", cb[ps_, d, 1, q * 16:(q + 1) * 16], ctmp[ps_, 2, :], -1.0, ctmp[ps_, 3, :], ALU.mult, ALU.subtract, [ctmp], [cb])
                ang, sn, cs_, xre, xim, wre, wim, tk = W
                RW = W + [Wi]
                SC = TWO_PI * (1.0 - 1e-6)
                ts("dve", ang[:], spos[d][:], th[d][:, j:j + 1], 1.0 / TWO_PI, ALU.mult, ALU.mult, [spos[d], th[d]] + RW, RW)
                cp("dve", Wi[:], ang[:], RW, RW)
                cp("dve", tk[:], Wi[:], RW, RW)
                tt("dve", ang[:], ang[:], tk[:], ALU.subtract, RW, RW)
                act(sn[:], ang[:], AF.Sin, RW, RW, scale=SC)
                ts("dve", tk[:], ang[:], 0.25, None, ALU.is_gt, None, RW, RW)
                stt("dve", ang[:], ang[:], 0.25, tk[:], ALU.add, ALU.subtract, RW, RW)
                act(cs_[:], ang[:], AF.Sin, RW, RW, scale=SC)
                for part, dst in ((0, xre), (1, xim)):
                    for ch in range(5):
                        cw2 = min(512, TOK - ch * 512)
                        mm(pf(ch)[:, 0:cw2], b_[:, part, :], uT[:, j // 4, ch * 512:ch * 512 + cw2], True, True, [b_, uT], [PB[ch]])
                    cp("act", dst[:], pf(0, 5)[:, 0:TOK], [PB[c2] for c2 in range(5)], RW)
                tt("dve", wre[:], xre[:], cs_[:], ALU.mult, RW, RW)
                tt("pool", tk[:], xim[:], sn[:], ALU.mult, RW, RW)
                tt("dve", wre[:], wre[:], tk[:], ALU.add, RW, RW)
                tt("dve", wim[:], xim[:], cs_[:], ALU.mult, RW, RW)
                tt("pool", tk[:], xre[:], sn[:], ALU.mult, RW, RW)
                tt("dve", wim[:], wim[:], tk[:], ALU.subtract, RW, RW)
                rb = rr[d][:, j:j + 1].to_broadcast([128, TOK])
                for src, dst in ((wre, xre), (wim, xim)):
                    if d == 0:
                        gen("dve", "tensor_tensor_scan", RW + [rr[d]], RW, out=dst[:], data0=rb, data1=src[:], initial=0.0, op0=ALU.mult, op1=ALU.add)
                    else:
                        gen("dve", "tensor_tensor_scan", RW + [rr[d]], RW, out=dst[:, 0:256][:, ::-1], data0=rb[:, 0:256], data1=src[:, 0:256][:, ::-1], initial=0.0, op0=ALU.mult, op1=ALU.add)
                        gen("dve", "tensor_tensor_scan", RW + [rr[d]], RW, out=dst[:, 256:TOK][:, ::-1], data0=rb[:, 0:2048], data1=src[:, 256:TOK][:, ::-1], initial=dst[:, 0:1], op0=ALU.mult, op1=ALU.add)
                hre = hb[2 * d]; him = hb[2 * d + 1]
                tt("dve", wre[:], xre[:], cs_[:], ALU.mult, RW, RW)
                tt("pool", tk[:], xim[:], sn[:], ALU.mult, RW, RW)
                tt("dve", hre[:], wre[:], tk[:], ALU.subtract, RW + [hre], RW + [hre])
                tt("dve", wim[:], xre[:], sn[:], ALU.mult, RW, RW)
                tt("pool", tk[:], xim[:], cs_[:], ALU.mult, RW, RW)
                tt("dve", him[:], wim[:], tk[:], ALU.add, RW + [him], RW + [him])
            for i in range(NT):
                bk = 5
                col = (i % 16) * 32
                q = 0
                for d in range(2):
                    for part in range(2):
                        mm(pf(bk)[:, col:col + 32], hb[2 * d + part][:, rows(i)], cb[:, d, part, :], q == 0, q == 3, [hb[2 * d + part], cb], [PB[bk]])
                        q += 1
                if i % 16 == 15 or i == NT - 1:
                    lo = (i // 16) * 16
                    nn = i - lo + 1
                    cp("act", ysb[:, lo:lo + nn, :], pf(bk)[:, 0:nn * 32].rearrange("p (a b) -> p a b", a=nn), [PB[bk]], [ysb])
            P.dma("pool", YF.t[:, j * 32:(j + 1) * 32].rearrange("(n p) c -> p n c", p=128), ysb[:], reads=[ysb], writes=[YF])
        A.release(m)
        m = A.mark()
        db = A.alloc("s5db", [1024], F32)
        gbb = A.alloc("s5gbb", [1024], F32)
        P.dma("sync", db[:], bcast(s5_d[li:li + 1, :]), writes=[db])
        P.dma("sync", gbb[:], bcast(s5_glu_b[li:li + 1, :]), writes=[gbb])
        yfs = [A.alloc(f"ryf{i}", [1024], F32) for i in range(2)]
        ufs = [A.alloc(f"ruf{i}", [1024], F32) for i in range(2)]
        gbs = [A.alloc(f"rgb{i}", [1024], BF16) for i in range(2)]
        for i in range(NT):
            yf = yfs[i % 2]; uf = ufs[i % 2]; gb_ = gbs[i % 2]
            P.dma("sync", yf[:], YF[rows(i), :], reads=[YF], writes=[yf])
            P.dma("sync", uf[:], PROJ[rows(i), 1536:2560], reads=[PROJ], writes=[uf])
            tt("dve", uf[:], uf[:], db[:], ALU.mult, [uf, db], [uf])
            tt("pool", yf[:], yf[:], uf[:], ALU.add, [yf, uf], [yf])
            act(gb_[:], yf[:], AF.Gelu, [yf], [gb_])
            P.dma("pool", GT5[rows(i), :], gb_[:], reads=[gb_], writes=[GT5])
        A.release(m)
        stage_transpose(GT5, GT5T, 1024)

        def epi_glu():
            gts = [A.alloc(f"gg{i}", [1024], BF16) for i in range(2)]
            sg = [A.alloc(f"gs{i}", [1024], F32) for i in range(2)]
            ob = [A.alloc(f"go{i}", [1024], BF16) for i in range(2)]
            gbb2 = A.alloc("gbb2", [1024], F32)
            P.dma("sync", gbb2[:], bcast(s5_glu_b[li:li + 1, :]), writes=[gbb2])
            st = {"n": 0}

            def epi(i, c0, gw, psap, pres):
                k = st["n"] % 2; st["n"] += 1
                P.dma("sync", gts[k][:], GT5[rows(i), :], reads=[GT5], writes=[gts[k]])
                tt("dve", sg[k][:, 0:gw], psap, gbb2[:, c0:c0 + gw], ALU.add, pres + [gbb2], [sg[k]])
                act(sg[k][:, 0:gw], sg[k][:, 0:gw], AF.Sigmoid, [sg[k]], [sg[k]])
                tt("dve", ob[k][:, 0:gw], sg[k][:, 0:gw], gts[k][:, c0:c0 + gw], ALU.mult, [sg[k], gts[k]], [ob[k]])
                P.dma("pool", MIX[rows(i), 1024 + c0:1024 + c0 + gw], ob[k][:, 0:gw], reads=[ob[k]], writes=[MIX])
            return epi
        stage_linear(GT5T, 8, s5_glu_w[li], 1024, epi_glu)

    def stage_attn_c(li):
        m = A.mark()
        qT = A.alloc("cqT", [16, TOK], BF16, parts=64)
        kT = A.alloc("ckT", [2, TOK], BF16, parts=64)
        vC = A.alloc("cvC", [NT, 128], BF16)
        sinkb = A.alloc("sinkb", [16], F32)
        maskw = A.alloc("maskw", [384], F32)
        P.dma("sync", sinkb[:], bcast(c_sink[li:li + 1, :]), writes=[sinkb])
        P.dma("sync", maskw[:], k_maskw, writes=[maskw])
        m1 = A.mark()
        qks = [A.alloc(f"cqk{i}", [18, 64], F32) for i in range(2)]
        qb = A.alloc("cqb", [18, 64], BF16)
        cs = [A.alloc(f"ccs{i}", [2, 32], F32) for i in range(2)]
        t1 = A.alloc("crt1", [18, 32], F32); t2 = A.alloc("crt2", [18, 32], F32)
        vf = [A.alloc(f"cvf{i}", [128], F32) for i in range(2)]
        for i in range(NT):
            qk = qks[i % 2]
            P.dma("sync", qk[:].rearrange("p a b -> p (a b)"), PROJ[rows(i), 0:1152], reads=[PROJ], writes=[qk])
            P.dma("sync", vf[i % 2][:], PROJ[rows(i), 1152:1280], reads=[PROJ], writes=[vf[i % 2]])
            cp("pool", vC[:, i, :], vf[i % 2][:], [vf[i % 2]], [vC])
            if i >= 2:
                c = cs[i % 2]
                P.dma("sync", c[:, 0, :], k_rope[2, rows(i - 2), 0:32], writes=[c])
                P.dma("sync", c[:, 1, :], k_rope[3, rows(i - 2), 0:32], writes=[c])
                cb = c[:, 0, :].unsqueeze(1).to_broadcast([128, 18, 32])
                sb = c[:, 1, :].unsqueeze(1).to_broadcast([128, 18, 32])
                x1 = qk[:, :, 0:32]; x2 = qk[:, :, 32:64]
                tt("dve", t1[:], x1, cb, ALU.mult, [qk, c], [t1])
                tt("pool", t2[:], x2, sb, ALU.mult, [qk, c], [t2])
                tt("dve", qb[:, :, 0:32], t1[:], t2[:], ALU.subtract, [t1, t2], [qb])
                tt("dve", t1[:], x1, sb, ALU.mult, [qk, c, qb], [t1])
                tt("pool", t2[:], x2, cb, ALU.mult, [qk, c, qb], [t2])
                tt("dve", qb[:, :, 32:64], t1[:], t2[:], ALU.add, [t1, t2], [qb])
            else:
                cp("dve", qb[:], qk[:], [qk], [qb])
            for q in range(3):
                bk = 5 + q
                nb = 8 if q < 2 else 2
                for jj in range(nb):
                    h = q * 8 + jj
                    tr(pbf(bk)[0:64, jj * 128:(jj + 1) * 128], qb[:, h, :], ident_bf[:], [qb, ident_bf], [PB[bk]])
                if q < 2:
                    cp("act" if q == 0 else "dve", qT[:, q * 8:(q + 1) * 8, rows(i)], pbf(bk)[0:64, :].rearrange("p (a b) -> p a b", a=8), [PB[bk]], [qT])
                else:
                    cp("dve", kT[:, :, rows(i)], pbf(bk)[0:64, 0:256].rearrange("p (a b) -> p a b", a=2), [PB[bk]], [kT])
        A.release(m1)
        Sm = A.alloc("cSm", [640], F32)
        pbs = [A.alloc(f"cpb{i}", [640], BF16) for i in range(2)]
        pT = A.alloc("cpT", [5, 128], BF16)
        sm = A.alloc("csm", [8], F32)
        omix = [A.alloc(f"comix{i}", [1024], BF16) for i in range(2)]
        n = 0
        for i in range(NT):
            om = omix[i % 2]
            if i >= 2:
                l_ = i - 2
                lo = max(l_ - 1, 0); hi = min(l_ + 1, 15)
                w = (hi - lo + 1) * 128
                k0 = 256 + lo * 128
                mcol = 128 if l_ == 0 else 0
            else:
                w = 0
            nk = 256 + w
            nkt = nk // 128
            for h in range(16):
                g = h // 8
                pb = pbs[n % 2]; n += 1
                mm(pf(0)[:, 0:256], qT[:, h, rows(i)], kT[:, g, 0:256], True, True, [qT, kT], [PB[0]])
                act(Sm[:, 0:256], pf(0)[:, 0:256], AF.Copy, [PB[0]], [Sm], scale=0.125)
                if w:
                    mm(pf(1)[:, 0:w], qT[:, h, rows(i)], kT[:, g, k0:k0 + w], True, True, [qT, kT], [PB[1]])
                    stt("dve", Sm[:, 256:256 + w], pf(1)[:, 0:w], 0.125, maskw[:, mcol:mcol + w], ALU.mult, ALU.add, [PB[1], maskw], [Sm])
                gen("dve", "reduce_max", [Sm], [sm], out=sm[:, 0:1], in_=Sm[:, 0:nk], axis=AX.X)
                tt("dve", sm[:, 0:1], sm[:, 0:1], sinkb[:, h:h + 1], ALU.max, [sm, sinkb], [sm])
                ts("dve", sm[:, 1:2], sm[:, 0:1], -1.0, None, ALU.mult, None, [sm], [sm])
                act(pb[:, 0:nk], Sm[:, 0:nk], AF.Exp, [Sm, sm], [pb, sm], bias=sm[:, 1:2], accum=sm[:, 2:3])
                act(sm[:, 4:5], sinkb[:, h:h + 1], AF.Exp, [sinkb, sm], [sm], bias=sm[:, 1:2])
                tt("dve", sm[:, 2:3], sm[:, 2:3], sm[:, 4:5], ALU.add, [sm], [sm])
                recip(sm[:, 3:4], sm[:, 2:3], [sm], [sm])
                bk = 6 + h % 2
                for kt_ in range(nkt):
                    tr(pbf(bk)[:, kt_ * 128:(kt_ + 1) * 128], pb[:, kt_ * 128:(kt_ + 1) * 128], ident_bf[:], [pb, ident_bf], [PB[bk]])
                cp("act" if h % 2 == 0 else "dve", pT[:, 0:nkt, :], pbf(bk)[:, 0:nkt * 128].rearrange("p (a b) -> p a b", a=nkt), [PB[bk]], [pT])
                for kt_ in range(nkt):
                    tile_ = kt_ if kt_ < 2 else 2 + lo + (kt_ - 2)
                    mm(pf(5)[:, 0:64], pT[:, kt_, :], vC[:, tile_, g * 64:(g + 1) * 64], kt_ == 0, kt_ == nkt - 1, [pT, vC], [PB[5]])
                act(om[:, h * 64:(h + 1) * 64], pf(5)[:, 0:64], AF.Copy, [PB[5], sm], [om], scale=sm[:, 3:4])
            P.dma("pool", MIX[rows(i), 0:1024], om[:], reads=[om], writes=[MIX])
        A.release(m)

    def stage_mamba(li, upto=99):
        m = A.mark()
        wb = [A.alloc(f"mcw{i}", [1536], F32) for i in range(4)]
        for k in range(3):
            P.dma("sync", wb[k][:], bcast(m_conv_w[li, k:k + 1, :]), writes=[wb[k]])
        P.dma("sync", wb[3][:], bcast(m_conv_b[li:li + 1, :]), writes=[wb[3]])
        dtb = A.alloc("mdtb", [32], F32); ab = A.alloc("mab", [32], F32)
        P.dma("sync", dtb[:], bcast(m_dt_bias[li:li + 1, :]), writes=[dtb])
        P.dma("sync", ab[:], bcast(m_a_log[li:li + 1, :]), writes=[ab])
        act(ab[:], ab[:], AF.Exp, [ab], [ab])
        ts("dve", ab[:], ab[:], -1.0, None, ALU.mult, None, [ab], [ab])
        xms = [A.alloc(f"mxm{i}", [1536], F32) for i in range(2)]
        xps = [A.alloc(f"mxp{i}", [1536], F32) for i in range(2)]
        xns = [A.alloc(f"mxn{i}", [1536], F32) for i in range(2)]
        acc = A.alloc("macc", [1536], F32); tq = A.alloc("mtq", [1536], F32)
        xcb = [A.alloc(f"mxcb{i}", [1536], BF16) for i in range(2)]
        dts = [A.alloc(f"mdt{i}", [64], F32) for i in range(2)]
        for i in range(NT):
            r0 = i * 128
            seg_lo = 0 if i < 2 else 256
            seg_hi = 256 if i < 2 else TOK
            xm = xms[i % 2]; xp = xps[i % 2]; xn = xns[i % 2]; xc_ = xcb[i % 2]; dt_ = dts[i % 2]
            P.dma("sync", xm[:], PROJ[r0:r0 + 128, 2304:3840], reads=[PROJ], writes=[xm])
            if r0 == seg_lo:
                gen("pool", "memset", [], [xp], ap=xp[:], constant=0.0)
                P.dma("sync", xp[1:128, :], PROJ[r0:r0 + 127, 2304:3840], reads=[PROJ], writes=[xp])
            else:
                P.dma("sync", xp[:], PROJ[r0 - 1:r0 + 127, 2304:3840], reads=[PROJ], writes=[xp])
            if r0 + 128 == seg_hi:
                gen("pool", "memset", [], [xn], ap=xn[:], constant=0.0)
                P.dma("sync", xn[0:127, :], PROJ[r0 + 1:r0 + 128, 2304:3840], reads=[PROJ], writes=[xn])
            else:
                P.dma("sync", xn[:], PROJ[r0 + 1:r0 + 129, 2304:3840], reads=[PROJ], writes=[xn])
            tt("dve", acc[:], xm[:], wb[1][:], ALU.mult, [xm, wb[1]], [acc])
            tt("pool", tq[:], xp[:], wb[0][:], ALU.mult, [xp, wb[0]], [tq])
            tt("dve", acc[:], acc[:], tq[:], ALU.add, [acc, tq], [acc])
            tt("pool", tq[:], xn[:], wb[2][:], ALU.mult, [xn, wb[2], acc], [tq])
            tt("dve", acc[:], acc[:], tq[:], ALU.add, [acc, tq], [acc])
            tt("dve", acc[:], acc[:], wb[3][:], ALU.add, [acc, wb[3]], [acc])
            act(xc_[:], acc[:], AF.Silu, [acc], [xc_])
            P.dma("pool", XC[rows(i), :], xc_[:], reads=[xc_], writes=[XC])
            P.dma("sync", dt_[:, 0:32], PROJ[rows(i), 3840:3872], reads=[PROJ], writes=[dt_])
            tt("dve", dt_[:, 0:32], dt_[:, 0:32], dtb[:], ALU.add, [dt_, dtb], [dt_])
            act(dt_[:, 0:32], dt_[:, 0:32], AF.Exp, [dt_], [dt_])
            act(dt_[:, 0:32], dt_[:, 0:32], AF.Ln, [dt_], [dt_], bias=1.0)
            tt("dve", dt_[:, 32:64], dt_[:, 0:32], ab[:], ALU.mult, [dt_, ab], [dt_])
            P.dma("pool", DTD[rows(i), :], dt_[:], reads=[dt_], writes=[DTD])
            import os as _os2
            if not _os2.environ.get("NO_SZ"):
                P.dma("sync", acc[:, 0:1024], PROJ[rows(i), 1280:2304], reads=[PROJ, xc_], writes=[acc])
                act(tq[:, 0:1024], acc[:, 0:1024], AF.Silu, [acc], [tq])
                P.dma("pool", SZ[rows(i), :], tq[:, 0:1024], reads=[tq], writes=[SZ])
        A.release(m)
        if upto < 2:
            return
        m = A.mark()
        H = A.alloc("mH", [2, 512], F32); Hb = A.alloc("mHb", [2, 512], BF16)
        TRI = [A.alloc(f"mtri{d}", [128], F32) for d in range(2)]
        NEGM = [A.alloc(f"mneg{d}", [128], F32) for d in range(2)]
        onesf = A.alloc("monesf", [128], F32)
        for d in range(2):
            P.dma("sync", TRI[d][:], k_tri[d], writes=[TRI[d]])
            P.dma("sync", NEGM[d][:], k_tri[2 + d], writes=[NEGM[d]])
        gen("dve", "memset", [], [onesf], ap=onesf[:], constant=1.0)
        warm = A.alloc("mwarm", [8], F32)
        gen("dve", "memset", [], [warm], ap=warm[:], constant=0.0)
        act(warm[:], warm[:], AF.Exp, [warm], [warm])
        Db = A.alloc("mDb", [16], F32); ngb = A.alloc("mngb", [1024], F32)
        P.dma("sync", Db[:], bcast(m_d[li:li + 1, :]), writes=[Db])
        P.dma("sync", ngb[:], bcast(m_norm_g[li:li + 1, :]), writes=[ngb])
        xss = [A.alloc(f"mxs{i}", [24, 64], BF16) for i in range(2)]
        dtd = [A.alloc(f"mdtd{i}", [64], F32) for i in range(2)]
        rhsM = A.alloc("mrhsM", [16, 128], F32)
        acs = A.alloc("macs", [16], F32); eacs = A.alloc("meacs", [16], F32); cdb = A.alloc("mcdb", [16], F32)
        seg = A.alloc("mseg", [16, 128], F32)
        BCT = A.alloc("mBCT", [4, 128], BF16)
        Wm = A.alloc("mW", [16, 128], BF16)
        xdt = A.alloc("mxdt", [16, 64], BF16); xdd = A.alloc("mxdd", [16, 64], BF16)
        yb = [A.alloc(f"my{i}", [16, 64], F32) for i in range(2)]
        yfl = [A.alloc(f"myf{i}", [1024], F32) for i in range(2)]
        zf = [A.alloc(f"mzf{i}", [1024], F32) for i in range(2)]
        t2 = A.alloc("mt2", [16, 64], F32)
        junk = A.alloc("mjunk", [1024], F32)
        ss = A.alloc("mss", [2], F32)
        ob = [A.alloc(f"mob{i}", [1024], BF16) for i in range(2)]
        n = 0
        import os as _os
        for d in [int(ch) for ch in _os.environ.get('MAMBA_DIRS', '01')]:
            order = list(range(NT)) if d == 0 else [1, 0] + list(range(NT - 1, 1, -1))
            LAST = 127 if d == 0 else 0
            gen("dve", "memset", [], [H], ap=H[:], constant=0.0)
            gen("dve", "memset", [], [Hb], ap=Hb[:], constant=0.0)
            for c in order:
                xs_ = xss[n % 2]; dd = dtd[n % 2]; y_ = yb[n % 2]; yf_ = yfl[n % 2]; z_ = zf[n % 2]; o_ = ob[n % 2]
                n += 1
                P.dma("sync", xs_[:].rearrange("p a b -> p (a b)"), XC[rows(c), :], reads=[XC], writes=[xs_])
                P.dma("sync", dd[:], DTD[rows(c), :], reads=[DTD], writes=[dd])
                xs3 = xs_[:, 0:16, :]
                Bb = xs_[:, 16:20, :].rearrange("p (g a) b -> p g (a b)", g=2)
                Cb = xs_[:, 20:24, :].rearrange("p (g a) b -> p g (a b)", g=2)
                dt_ = dd[:, d * 16:(d + 1) * 16]
                dA = dd[:, 32 + d * 16:32 + (d + 1) * 16]
                tt("dve", rhsM[:], TRI[d][:].unsqueeze(1).to_broadcast([128, 16, 128]), dA.unsqueeze(2).to_broadcast([128, 16, 128]), ALU.mult, [TRI[d], dd], [rhsM])
                for q in range(4):
                    mm(pf(q), onesf[:], rhsM[:, 4 * q:4 * q + 4, :].rearrange("p a b -> p (a b)"), True, True, [onesf, rhsM], [PB[q]])
                mm(pf(4)[:, 0:16], TRI[d][:], dA, True, True, [TRI[d], dd], [PB[4]])
                cp("act", acs[:], pf(4)[:, 0:16], [PB[4]], [acs])
                acsb = pf(0, 4).rearrange("p (a b) -> p a b", a=16)
                tt("dve", seg[:], acsb, acs[:].unsqueeze(2).to_broadcast([128, 16, 128]), ALU.subtract, [PB[0], PB[1], PB[2], PB[3], acs], [seg])
                act(cdb[:], acsb[:, :, LAST], AF.Exp, [PB[0], PB[1], PB[2], PB[3]], [cdb])
                tt("pool", seg[:], seg[:], NEGM[d][:].unsqueeze(1).to_broadcast([128, 16, 128]), ALU.add, [seg, NEGM[d]], [seg])
                act(seg[:], seg[:], AF.Exp, [seg], [seg])
                act(eacs[:], acs[:], AF.Exp, [acs], [eacs])
                if upto < 3:
                    continue
                for q in range(2):
                    for g in range(2):
                        src = Bb if q == 0 else Cb
                        tr(pbf(6)[:, (q * 2 + g) * 128:(q * 2 + g + 1) * 128], src[:, g, :], ident_bf[:], [xs_, ident_bf], [PB[6]])
                cp("dve", BCT[:], pbf(6)[:, 0:512].rearrange("p (a b) -> p a b", a=4), [PB[6]], [BCT])
                for g in range(2):
                    mm(pf(5)[:, g * 128:(g + 1) * 128], BCT[:, g, :], BCT[:, 2 + g, :], True, True, [BCT], [PB[5]])
                for g in range(2):
                    tt("dve", Wm[:, g * 8:(g + 1) * 8, :], seg[:, g * 8:(g + 1) * 8, :], pf(5)[:, g * 128:(g + 1) * 128].unsqueeze(1).to_broadcast([128, 8, 128]), ALU.mult, [seg, PB[5]], [Wm])
                if upto < 4:
                    continue
                tt("pool", xdt[:], xs3, dt_.unsqueeze(2).to_broadcast([128, 16, 64]), ALU.mult, [xs_, dd], [xdt])
                tt("pool", xdd[:], xdt[:], seg[:, :, LAST].unsqueeze(2).to_broadcast([128, 16, 64]), ALU.mult, [xdt, seg], [xdd])
                for h in range(16):
                    bk = h // 8
                    mm(pf(bk)[:, (h % 8) * 64:(h % 8 + 1) * 64], Wm[:, h, :], xdt[:, h, :], True, True, [Wm, xdt], [PB[bk]])
                for g in range(2):
                    mm(pf(2 + g), BCT[:, 2 + g, :], Hb[:, g, :], True, True, [BCT, Hb], [PB[2 + g]])
                tt("dve", y_[:], pf(2, 2).rearrange("p (a b) -> p a b", a=16), eacs[:].unsqueeze(2).to_broadcast([128, 16, 64]), ALU.mult, [PB[2], PB[3], eacs], [y_])
                tt("dve", y_[:], y_[:], pf(0, 2).rearrange("p (a b) -> p a b", a=16), ALU.add, [y_, PB[0], PB[1]], [y_])
                if upto < 5:
                    continue
                sbk = [4, 7]
                for g in range(2):
                    mm(pf(sbk[g]), Bb[:, g, :], xdd[:, g * 8:(g + 1) * 8, :].rearrange("p a b -> p (a b)"), True, True, [xs_, xdd], [PB[sbk[g]]])
                H4 = H[:].rearrange("p g (a b) -> p g a b", a=8)
                for g in range(2):
                    tt("dve", H4[:, g], H4[:, g], cdb[:, g * 8:(g + 1) * 8].unsqueeze(2).to_broadcast([128, 8, 64]), ALU.mult, [H, cdb, Hb], [H])
                    tt("dve", H[:, g, :], H[:, g, :], pf(sbk[g]), ALU.add, [H, PB[sbk[g]]], [H])
                cp("pool", Hb[:], H[:], [H], [Hb])
                if upto < 6:
                    continue
                P.dma(_os.environ.get("MAMBA_STQ", "sync"), (YF if d == 0 else YB)[rows(c), :], y_[:].rearrange("p a b -> p (a b)"), reads=[y_], writes=[YF if d == 0 else YB])
        A.release(m)
        if upto < 7:
            return
        m = A.mark()
        Db = A.alloc("rDb", [16], F32); ngb = A.alloc("rngb", [1024], F32)
        P.dma("sync", Db[:], bcast(m_d[li:li + 1, :]), writes=[Db])
        P.dma("sync", ngb[:], bcast(m_norm_g[li:li + 1, :]), writes=[ngb])
        yfs = [A.alloc(f"ryf{i}", [1024], F32) for i in range(2)]
        ybs = [A.alloc(f"ryb{i}", [1024], F32) for i in range(2)]
        zs = [A.alloc(f"rz{i}", [1024], F32) for i in range(2)]
        xsb = [A.alloc(f"rxs{i}", [1024], BF16) for i in range(2)]
        t2 = A.alloc("rt2", [16, 64], F32)
        junk = A.alloc("rjunk", [1024], F32)
        sss = [A.alloc(f"rss{i}", [2], F32) for i in range(2)]
        obs = [A.alloc(f"rob{i}", [1024], BF16) for i in range(2)]
        for i in range(NT):
            yf_ = yfs[i % 2]; yb_ = ybs[i % 2]; z_ = zs[i % 2]; xs_ = xsb[i % 2]; ss = sss[i % 2]; o_ = obs[i % 2]
            P.dma("sync", yf_[:], YF[rows(i), :], reads=[YF], writes=[yf_])
            P.dma("sync", yb_[:], YB[rows(i), :], reads=[YB], writes=[yb_])
            P.dma("sync", z_[:], SZ[rows(i), :], reads=[SZ], writes=[z_])
            P.dma("sync", xs_[:], XC[rows(i), 0:1024], reads=[XC], writes=[xs_])
            tt("dve", yf_[:], yf_[:], yb_[:], ALU.add, [yf_, yb_], [yf_])
            tt("pool", t2[:], xs_[:].rearrange("p (a b) -> p a b", a=16), Db[:].unsqueeze(2).to_broadcast([128, 16, 64]), ALU.mult, [xs_, Db], [t2])
            tt("dve", yf_[:], yf_[:], t2[:].rearrange("p a b -> p (a b)"), ALU.add, [yf_, t2], [yf_])
            tt("dve", yf_[:], yf_[:], z_[:], ALU.mult, [yf_, z_], [yf_])
            act(junk[:], yf_[:], AF.Square, [yf_], [junk, ss], accum=ss[:, 0:1])
            ts("dve", ss[:, 1:2], ss[:, 0:1], 1.0 / 1024, EPS, ALU.mult, ALU.add, [ss], [ss])
            act(ss[:, 1:2], ss[:, 1:2], AF.Sqrt, [ss], [ss])
            recip(ss[:, 1:2], ss[:, 1:2], [ss], [ss])
            stt("dve", o_[:], yf_[:], ss[:, 1:2], ngb[:], ALU.mult, ALU.mult, [yf_, ss, ngb], [o_])
            P.dma("sync", MIX[rows(i), 1024:2048], o_[:], reads=[o_], writes=[MIX])
        A.release(m)

    dbg_outs = {}

    def dump(name, src, shape, dtype):
        o = nc.dram_tensor("dbg_" + name, list(shape), dtype, kind="ExternalOutput").ap()
        P.barrier()
        P.dma("sync", o, src, reads=[])
        P.barrier()

    if unit == "mamba":
        proj_in = inp("proj_in", [TOK, 3872])
        for i in range(NT):
            P.dma("sync", PROJ[rows(i), :], proj_in[rows(i), :], writes=[PROJ])
        P.barrier()
        stage_mamba(0, upto)
        P.barrier()
        if upto >= 9:
            dump("mix", MIX.t[:, 1024:2048], [TOK, 1024], BF16)
        dump("xc", XC.t, [TOK, 1536], BF16)
        dump("dtd", DTD.t, [TOK, 64], F32)
        if upto >= 6:
            dump("yf", YF.t, [TOK, 1024], F32)
        P.emit()
        return nc
    for l in range(n_layers):
        li = l // 2
        stage_mod(l)
        stage_norm(norm_g[2 * l:2 * l + 1, :], 0, 1, HT, None)
        P.barrier()
        if l % 2 == 0:
            stage_linear(HT, 16, ev_w_in[li], 2560, epi_store(PROJ))
            P.barrier()
            if dbg and l == dbg.get("layer", -1) and "proj" in dbg["what"]:
                dump("proj", PROJ.t, [TOK, 3872], F32)
            stage_attn_a(li)
            stage_s5(li)
            w_out = ev_w_out[li]
        else:
            stage_linear(HT, 16, od_w_in[li], 3872, epi_store(PROJ))
            P.barrier()
            import os
            if not os.environ.get('SKIP_C'):
                stage_attn_c(li)
            if not os.environ.get('SKIP_M'):
                stage_mamba(li)
            w_out = od_w_out[li]
        P.barrier()
        if dbg and l == dbg.get("layer", -1) and "mix" in dbg["what"]:
            dump("mix", MIX.t, [TOK, D], BF16)
        stage_transpose(MIX, MIXT, 2048)
        stage_linear(MIXT, 16, w_out, 2048, epi_resid(2))
        P.barrier()
        if dbg and l == dbg.get("layer", -1) and "xmid" in dbg["what"]:
            dump("xmid", X.t, [TOK, D], F32)
        stage_norm(norm_g[2 * l + 1:2 * l + 2, :], 3, 4, HT, HTOK)
        P.barrier()
        stage_moe(l)
        P.barrier()
        if dbg and l == dbg.get("layer", -1) and "xout" in dbg["what"]:
            dump("xout", X.t, [TOK, D], F32)
    stage_norm(final_norm_g[0:1, :], 0, 0, None, None, tiles=range(2, NT), final_out=y_out)
    P.emit()
    return nc


def host_consts():
    c = {}
    c["k_ident"] = np.eye(128, dtype=np.float32)
    c["k_iota"] = np.tile(np.arange(256, dtype=np.float32)[None, :], (128, 1))
    p = np.arange(128, dtype=np.float32)
    c["k_iotap"] = np.stack([p, p + 128], axis=1).astype(np.float32)
    t = np.arange(TOK, dtype=np.float32)
    sb = np.where(t < 256, 255 - t, 256 + (2303 - t)).astype(np.float32)
    c["k_spos"] = np.stack([np.tile(t[None], (128, 1)), np.tile(sb[None], (128, 1))]).astype(np.float32)
    rope = np.zeros((4, 2048, 64), np.float32)
    tt_ = np.arange(2048)
    row = (tt_ // 64).astype(np.float32); col = (tt_ % 64).astype(np.float32)
    for idx, hd in ((0, 128), (2, 64)):
        nf = hd // 4
        inv = (10000.0 ** (-np.arange(nf, dtype=np.float32) / nf)).astype(np.float32)
        ang = np.concatenate([row[:, None] * inv, col[:, None] * inv], axis=-1).astype(np.float32)
        rope[idx, :, :hd // 2] = np.cos(ang)
        rope[idx + 1, :, :hd // 2] = np.sin(ang)
    c["k_rope"] = rope
    r = np.arange(128)[:, None]; j = np.arange(128)[None, :]
    mw = np.zeros((128, 384), np.float32)
    mw[:, 0:128] = np.where(j >= r, 0.0, -1e30)
    mw[:, 256:384] = np.where(j <= r, 0.0, -1e30)
    c["k_maskw"] = mw
    s = np.arange(128)[:, None]; i = np.arange(128)[None, :]
    tri = np.zeros((4, 128, 128), np.float32)
    tri[0] = (s <= i); tri[1] = (s >= i)
    tri[2] = np.where(s <= i, 0.0, -1e30); tri[3] = np.where(s >= i, 0.0, -1e30)
    c["k_tri"] = tri
    return c


def kernel(**inputs):
    inp = {k: np.asarray(v) for k, v in inputs.items()}
    nc = build_program()
    shared = host_consts()
    f = lambda a: np.ascontiguousarray(a, dtype=np.float32)
    for k in ["mod_w", "mod_b", "ev_w_in", "ev_w_out", "a_q_norm", "a_k_norm", "s5_d", "s5_glu_w", "s5_glu_b", "od_w_in", "od_w_out",
              "c_sink", "m_conv_w", "m_conv_b", "m_d", "m_norm_g", "moe_router", "moe_w_gate", "moe_w_up", "moe_w_down"]:
        shared[k] = f(inp[k])
    shared["norm_g"] = f(inp["norm_g"].reshape(8, D))
    shared["m_dt_bias"] = f(inp["m_dt_bias"].reshape(2, 32))
    shared["m_a_log"] = f(inp["m_a_log"].reshape(2, 32))
    shared["final_norm_g"] = f(inp["final_norm_g"].reshape(1, D))
    are = inp["s5_a_re"].reshape(2, 2, 32, 2, 64)
    aim = inp["s5_a_im"].reshape(2, 2, 32, 2, 64)
    ldt = np.broadcast_to(inp["s5_log_dt"].reshape(2, 2, 32, 2, 1), (2, 2, 32, 2, 64))
    par = np.stack([are, aim, ldt], axis=2)
    shared["s5_par"] = f(par.transpose(0, 1, 4, 5, 2, 3).reshape(2, 2, 128, 96))
    bblk = np.zeros((2, 2, 2, 32, 128, 128), np.float32)
    for part, key in ((0, "s5_b_re"), (1, "s5_b_im")):
        b = inp[key].reshape(2, 2, 32, 2, 64, 16)
        for jj in range(32):
            for q in range(2):
                u0 = 32 * (jj % 4) + q * 16
                bblk[:, :, part, jj, u0:u0 + 16, q * 64:(q + 1) * 64] = b[:, :, jj, q].transpose(0, 1, 3, 2)
    shared["s5_bblk"] = bblk
    cb = np.stack([inp["s5_c_re"], inp["s5_c_im"]], axis=2).reshape(2, 2, 2, 32, 2, 16, 64)
    shared["s5_cblk"] = f(cb.transpose(0, 1, 2, 4, 6, 3, 5).reshape(2, 2, 2, 128, 32, 16))
    in_maps = []
    for b in range(8):
        mp = dict(shared)
        mp["x_in"] = f(inp["x"][b]); mp["ctx_in"] = f(inp["ctx"][b])
        cT = np.stack([inp["c"][b].reshape(16, 128).T, inp["c_ctx"].reshape(16, 128).T], axis=-1)
        mp["cT_in"] = f(cT.reshape(128, 32))
        in_maps.append(mp)
    res = run_bass_kernel_spmd(nc, in_maps, core_ids=list(range(8)))
    return np.stack([r["y_out"] for r in res.results], axis=0).astype(np.float32)
```
